# Optimizing a Trainium2 kernel written in Bass

```python
import math
import jax, jax.numpy as jnp
from jax import lax
import numpy as np

D_MODEL = 1024
BATCH = 2
SEQ = 8192
DEPTH = 2

GRID_W = 64
CTX_LEN = 256
N_MIXERS = 2
N_SSD_LAYERS = (DEPTH + 1) // 2
N_ATTN_LAYERS = DEPTH // 2
ALPHA = (2.0 * DEPTH) ** 0.25
BETA = (8.0 * DEPTH) ** -0.25
LN_EPS = 1e-5
RMS_EPS = 1e-5

SSD_EXPAND = 2
D_INNER = SSD_EXPAND * D_MODEL
SSD_HEAD_DIM = 64
SSD_HEADS = D_INNER // SSD_HEAD_DIM
SSD_GROUPS = 4
SSD_HEADS_PER_GROUP = SSD_HEADS // SSD_GROUPS
D_STATE = 128
CONV_W = 3
CONV_DIM = D_INNER + 2 * SSD_GROUPS * D_STATE
SSD_IN_DIM = D_INNER + CONV_DIM + 2 * SSD_HEADS
CHUNK = 128

ATTN_HEADS = 16
ATTN_KV_HEADS = 4
ATTN_GROUP = ATTN_HEADS // ATTN_KV_HEADS
ATTN_HEAD_DIM = 64
Q_DIM = ATTN_HEADS * ATTN_HEAD_DIM
KV_DIM = ATTN_KV_HEADS * ATTN_HEAD_DIM
WINDOW = 128
ATTN_BLOCK = 128
ROPE_BASE = 10000.0

N_EXPERTS = 32
TOP_K = 4
D_FF = D_MODEL
SWIGLU_ALPHA = 1.702
SWIGLU_LIMIT = 7.0

kernel_name = 'hybrid_ssd_swa_moe_diffusion_trunk'


def layer_norm(t, g, b):
    tf = t.astype(jnp.float32)
    mu = jnp.mean(tf, axis=-1, keepdims=True)
    var = jnp.mean(jnp.square(tf - mu), axis=-1, keepdims=True)
    return ((tf - mu) * lax.rsqrt(var + LN_EPS)).astype(t.dtype) * g + b


def centred_depthwise_conv(u, w):
    return lax.conv_general_dilated(
        u, w[:, None, :].astype(u.dtype), window_strides=(1,),
        padding=[(CONV_W // 2, CONV_W // 2)],
        dimension_numbers=('NWC', 'WIO', 'NWC'), feature_group_count=u.shape[-1])


def ssd_scan(x, dt, a, b_in, c_in, init_state):
    bsz, n = x.shape[0], x.shape[1]
    nc = n // CHUNK
    f32 = jnp.float32
    G, HG, P, N = SSD_GROUPS, SSD_HEADS_PER_GROUP, SSD_HEAD_DIM, D_STATE
    xdt = (x.astype(f32) * dt[..., None]).reshape(bsz, nc, CHUNK, G, HG, P)
    bc = b_in.astype(f32).reshape(bsz, nc, CHUNK, G, N)
    cc = c_in.astype(f32).reshape(bsz, nc, CHUNK, G, N)
    to_ghc = lambda t: jnp.transpose(t, (0, 3, 4, 1, 2))
    to_clg = lambda t: jnp.transpose(t, (0, 3, 4, 1, 2))
    cs = jnp.cumsum(to_ghc((dt * a).reshape(bsz, nc, CHUNK, G, HG)), axis=-1)
    tril = jnp.tril(jnp.ones((CHUNK, CHUNK), dtype=bool))
    decay_in = jnp.exp(jnp.where(tril, cs[..., :, None] - cs[..., None, :], -jnp.inf))
    cb = jnp.einsum('bclgn,bcsgn->bgcls', cc, bc)
    y_diag = jnp.einsum('bghcls,bcsghp->bclghp', cb[:, :, None] * decay_in, xdt)
    decay_to_end = to_clg(jnp.exp(cs[..., -1:] - cs))
    chunk_states = jnp.einsum('bclgn,bclghp->bcghpn', bc, xdt * decay_to_end[..., None])
    chunk_decay = jnp.exp(cs[..., -1])

    def step(state, inp):
        dec, st = inp
        return state * dec[..., None, None] + st, state

    final_state, entry_states = lax.scan(
        step, init_state, (jnp.moveaxis(chunk_decay, -1, 0), jnp.moveaxis(chunk_states, 1, 0)))
    y_off = jnp.einsum('bclgn,cbghpn->bclghp', cc, entry_states) * to_clg(jnp.exp(cs))[..., None]
    y = (y_diag + y_off).reshape(bsz, n, G * HG, P)
    return y, final_state


def ssd_mixer(h_lat, h_ctx, w_in, conv_w, conv_b, dt_bias, a_log, d_skip, norm_w, w_out, need_ctx):
    f32 = jnp.float32
    a = -jnp.exp(a_log.astype(f32))
    gn = SSD_GROUPS * D_STATE

    def project(h):
        bsz, n = h.shape[0], h.shape[1]
        p = h @ w_in
        z = p[..., :D_INNER]
        xbc = jax.nn.silu(centred_depthwise_conv(p[..., D_INNER:D_INNER + CONV_DIM], conv_w) + conv_b)
        xs = xbc[..., :D_INNER].reshape(bsz, n, SSD_HEADS, SSD_HEAD_DIM)
        b_in = xbc[..., D_INNER:D_INNER + gn].reshape(bsz, n, SSD_GROUPS, D_STATE)
        c_in = xbc[..., D_INNER + gn:].reshape(bsz, n, SSD_GROUPS, D_STATE)
        dt = jax.nn.softplus(p[..., D_INNER + CONV_DIM:].astype(f32)
                             + dt_bias.reshape(-1).astype(f32)).reshape(bsz, n, 2, SSD_HEADS)
        return z, xs, b_in, c_in, dt

    def bidir(xs, b_in, c_in, dt, init_f, init_b):
        flip = lambda t: jnp.flip(t, axis=1)
        y_f, s_f = ssd_scan(xs, dt[:, :, 0], a[0], b_in, c_in, init_f)
        y_b, s_b = ssd_scan(flip(xs), flip(dt[:, :, 1]), a[1], flip(b_in), flip(c_in), init_b)
        return y_f + flip(y_b), s_f, s_b

    def finish(y, xs, z):
        bsz, n = z.shape[0], z.shape[1]
        y = y + xs.astype(f32) * d_skip.astype(f32)[:, None]
        g = y.reshape(bsz, n, D_INNER) * jax.nn.silu(z.astype(f32))
        g = g.reshape(bsz, n, SSD_GROUPS, D_INNER // SSD_GROUPS)
        g = g * lax.rsqrt(jnp.mean(g * g, axis=-1, keepdims=True) + RMS_EPS)
        g = g.reshape(bsz, n, D_INNER).astype(z.dtype) * norm_w
        return g @ w_out

    z_c, x_c, b_c, c_c, dt_c = project(h_ctx)
    z_l, x_l, b_l, c_l, dt_l = project(h_lat)
    zero = jnp.zeros((h_ctx.shape[0], SSD_GROUPS, SSD_HEADS_PER_GROUP, SSD_HEAD_DIM, D_STATE), f32)
    y_c, s_cf, s_cb = bidir(x_c, b_c, c_c, dt_c, zero, zero)
    y_l, _, _ = bidir(x_l, b_l, c_l, dt_l, s_cf, s_cb)
    y_lat = finish(y_l, x_l, z_l)
    y_ctx = finish(y_c, x_c, z_c) if need_ctx else None
    return y_lat, y_ctx


def axial_rope(n, dtype):
    rows = n // GRID_W
    row = jnp.repeat(jnp.arange(rows), GRID_W).astype(jnp.float32)
    col = jnp.broadcast_to(jnp.arange(GRID_W)[None, :], (rows, GRID_W)).reshape(-1).astype(jnp.float32)
    n_freq = ATTN_HEAD_DIM // 4
    inv = jnp.power(ROPE_BASE, -jnp.arange(n_freq, dtype=jnp.float32) / n_freq)
    ang = jnp.concatenate([row[:, None] * inv, col[:, None] * inv], axis=-1)
    return jnp.cos(ang).astype(dtype), jnp.sin(ang).astype(dtype)


def apply_rope(t, cos, sin):
    half = t.shape[-1] // 2
    t1, t2 = t[..., :half], t[..., half:]
    return jnp.concatenate([t1 * cos - t2 * sin, t2 * cos + t1 * sin], axis=-1)


def band_blocks(t, nb):
    n = t.shape[1]
    tp = jnp.pad(t, ((0, 0), (ATTN_BLOCK, ATTN_BLOCK), (0, 0), (0, 0)))
    parts = [tp[:, o:o + n].reshape(t.shape[0], nb, ATTN_BLOCK, t.shape[2], t.shape[3])
             for o in (0, ATTN_BLOCK, 2 * ATTN_BLOCK)]
    return jnp.concatenate(parts, axis=2)


def band_mask(nb, n):
    qi = jnp.arange(ATTN_BLOCK)[:, None]
    kj = jnp.arange(3 * ATTN_BLOCK)[None, :]
    in_window = jnp.abs(qi + ATTN_BLOCK - kj) <= WINDOW
    key_pos = jnp.arange(nb)[:, None] * ATTN_BLOCK - ATTN_BLOCK + jnp.arange(3 * ATTN_BLOCK)[None, :]
    in_range = (key_pos >= 0) & (key_pos < n)
    return in_window[None] & in_range[:, None, :]


def window_gqa_mixer(h_lat, h_ctx, w_qkv, b_qkv, sinks, w_o, b_o, cos, sin, need_ctx):
    f32 = jnp.float32
    bsz, n_lat = h_lat.shape[0], h_lat.shape[1]
    n_ctx = h_ctx.shape[1]
    scale = ATTN_HEAD_DIM ** -0.5

    def split_qkv(h):
        t = h @ w_qkv + b_qkv
        lead = t.shape[:-1]
        q = t[..., :Q_DIM].reshape(*lead, ATTN_KV_HEADS, ATTN_GROUP, ATTN_HEAD_DIM)
        k = t[..., Q_DIM:Q_DIM + KV_DIM].reshape(*lead, ATTN_KV_HEADS, ATTN_HEAD_DIM)
        v = t[..., Q_DIM + KV_DIM:].reshape(*lead, ATTN_KV_HEADS, ATTN_HEAD_DIM)
        return q, k, v

    q_l, k_l, v_l = split_qkv(h_lat)
    q_c, k_c, v_c = split_qkv(h_ctx)
    q_l = apply_rope(q_l, cos[:, None, None, :], sin[:, None, None, :])
    k_l = apply_rope(k_l, cos[:, None, :], sin[:, None, :])
    sink = sinks.reshape(ATTN_KV_HEADS, ATTN_GROUP).astype(f32)

    nb = n_lat // ATTN_BLOCK
    qb = q_l.reshape(bsz, nb, ATTN_BLOCK, ATTN_KV_HEADS, ATTN_GROUP, ATTN_HEAD_DIM)
    kb = band_blocks(k_l, nb)
    vb = band_blocks(v_l, nb)
    s_band = jnp.einsum('bnqkgd,bnskd->bnkgqs', qb, kb).astype(f32) * scale
    s_band = jnp.where(band_mask(nb, n_lat)[None, :, None, None], s_band, -jnp.inf)
    s_ctx = jnp.einsum('bnqkgd,bckd->bnkgqc', qb, k_c).astype(f32) * scale
    s_sink = jnp.broadcast_to(sink[None, None, :, :, None, None], s_ctx.shape[:-1] + (1,))
    p = jax.nn.softmax(jnp.concatenate([s_ctx, s_band, s_sink], axis=-1), axis=-1).astype(v_l.dtype)
    o_l = (jnp.einsum('bnkgqc,bckd->bnqkgd', p[..., :n_ctx], v_c)
           + jnp.einsum('bnkgqs,bnskd->bnqkgd', p[..., n_ctx:n_ctx + 3 * ATTN_BLOCK], vb))
    y_lat = o_l.reshape(bsz, n_lat, Q_DIM) @ w_o + b_o

    y_ctx = None
    if need_ctx:
        s = jnp.einsum('bqkgd,bckd->bkgqc', q_c, k_c).astype(f32) * scale
        s_sink_c = jnp.broadcast_to(sink[None, :, :, None, None], s.shape[:-1] + (1,))
        pc = jax.nn.softmax(jnp.concatenate([s, s_sink_c], axis=-1), axis=-1).astype(v_c.dtype)
        o_c = jnp.einsum('bkgqc,bckd->bqkgd', pc[..., :n_ctx], v_c)
        y_ctx = o_c.reshape(bsz, n_ctx, Q_DIM) @ w_o + b_o
    return y_lat, y_ctx


def clamped_swiglu(h):
    glu = jnp.minimum(h[..., ::2], SWIGLU_LIMIT)
    lin = jnp.clip(h[..., 1::2], -SWIGLU_LIMIT, SWIGLU_LIMIT)
    return glu * jax.nn.sigmoid(SWIGLU_ALPHA * glu) * (lin + 1.0)


def moe(t, w_r, b_r, w1, b1, w2, b2):
    logits = (t @ w_r + b_r).astype(jnp.float32)
    top_val, top_idx = lax.top_k(logits, TOP_K)
    top_w = jax.nn.softmax(top_val, axis=-1)
    gates = jnp.einsum('tk,tke->te', top_w,
                       jax.nn.one_hot(top_idx, N_EXPERTS, dtype=jnp.float32)).astype(t.dtype)
    out = jnp.zeros_like(t)
    for e in range(N_EXPERTS):
        y_e = clamped_swiglu(t @ w1[e] + b1[e]) @ w2[e] + b2[e]
        out = out + gates[:, e:e + 1] * y_e
    return out


def setup_inputs(seed: int = 0) -> dict:
    key = jax.random.key(seed)
    keys = iter(jax.random.split(key, 32))
    f32 = jnp.float32

    def nrm(shape, scale):
        return scale * jax.random.normal(next(keys), shape, f32)

    x = nrm((BATCH, SEQ, D_MODEL), 1.0)
    c = nrm((BATCH, D_MODEL), 1.0)
    ctx = nrm((BATCH, CTX_LEN, D_MODEL), 1.0)
    c_ctx = nrm((D_MODEL,), 1.0)
    ada_w = nrm((DEPTH, D_MODEL, 6 * D_MODEL), D_MODEL ** -0.5)
    ada_b = nrm((DEPTH, 6 * D_MODEL), 0.02)
    ln_g = 1.0 + nrm((DEPTH, 2, D_MODEL), 0.02)
    ln_b = nrm((DEPTH, 2, D_MODEL), 0.02)
    ssd_w_in = nrm((N_SSD_LAYERS, D_MODEL, SSD_IN_DIM), D_MODEL ** -0.5)
    ssd_conv_w = nrm((N_SSD_LAYERS, CONV_W, CONV_DIM), CONV_W ** -0.5)
    ssd_conv_b = nrm((N_SSD_LAYERS, CONV_DIM), 0.02)
    dt0 = jnp.exp(jax.random.uniform(next(keys), (N_SSD_LAYERS, 2, SSD_HEADS), f32,
                                     minval=math.log(1e-3), maxval=math.log(1e-1)))
    ssd_dt_bias = dt0 + jnp.log(-jnp.expm1(-dt0))
    ssd_a_log = jnp.log(jax.random.uniform(next(keys), (N_SSD_LAYERS, 2, SSD_HEADS), f32,
                                           minval=1.0, maxval=16.0))
    ssd_d = 1.0 + nrm((N_SSD_LAYERS, SSD_HEADS), 0.1)
    ssd_norm_w = 1.0 + nrm((N_SSD_LAYERS, D_INNER), 0.02)
    ssd_w_out = nrm((N_SSD_LAYERS, D_INNER, D_MODEL), BETA * D_INNER ** -0.5)
    attn_w_qkv = nrm((N_ATTN_LAYERS, D_MODEL, Q_DIM + 2 * KV_DIM), D_MODEL ** -0.5)
    attn_b_qkv = nrm((N_ATTN_LAYERS, Q_DIM + 2 * KV_DIM), 0.02)
    attn_sinks = nrm((N_ATTN_LAYERS, ATTN_HEADS), 0.5)
    attn_w_o = nrm((N_ATTN_LAYERS, Q_DIM, D_MODEL), BETA * Q_DIM ** -0.5)
    attn_b_o = nrm((N_ATTN_LAYERS, D_MODEL), 0.02)
    router_w = nrm((DEPTH, D_MODEL, N_EXPERTS), D_MODEL ** -0.5)
    router_b = nrm((DEPTH, N_EXPERTS), 0.01)
    moe_w1 = nrm((DEPTH, N_EXPERTS, D_MODEL, 2 * D_FF), D_MODEL ** -0.5)
    moe_b1 = nrm((DEPTH, N_EXPERTS, 2 * D_FF), 0.02)
    moe_w2 = nrm((DEPTH, N_EXPERTS, D_FF, D_MODEL), BETA * D_FF ** -0.5)
    moe_b2 = nrm((DEPTH, N_EXPERTS, D_MODEL), 0.02)
    return {'x': x, 'c': c, 'ctx': ctx, 'c_ctx': c_ctx, 'ada_w': ada_w, 'ada_b': ada_b,
            'ln_g': ln_g, 'ln_b': ln_b, 'ssd_w_in': ssd_w_in, 'ssd_conv_w': ssd_conv_w,
            'ssd_conv_b': ssd_conv_b, 'ssd_dt_bias': ssd_dt_bias, 'ssd_a_log': ssd_a_log,
            'ssd_d': ssd_d, 'ssd_norm_w': ssd_norm_w, 'ssd_w_out': ssd_w_out,
            'attn_w_qkv': attn_w_qkv, 'attn_b_qkv': attn_b_qkv, 'attn_sinks': attn_sinks,
            'attn_w_o': attn_w_o, 'attn_b_o': attn_b_o, 'router_w': router_w, 'router_b': router_b,
            'moe_w1': moe_w1, 'moe_b1': moe_b1, 'moe_w2': moe_w2, 'moe_b2': moe_b2}


def reference(x, c, ctx, c_ctx, ada_w, ada_b, ln_g, ln_b, ssd_w_in, ssd_conv_w, ssd_conv_b,
              ssd_dt_bias, ssd_a_log, ssd_d, ssd_norm_w, ssd_w_out, attn_w_qkv, attn_b_qkv,
              attn_sinks, attn_w_o, attn_b_o, router_w, router_b, moe_w1, moe_b1, moe_w2, moe_b2):
    bsz, n_lat = x.shape[0], x.shape[1]
    n_lat_tok = bsz * n_lat
    cos, sin = axial_rope(n_lat, x.dtype)
    act_c = jax.nn.silu(c)
    act_cc = jax.nn.silu(c_ctx)
    for i in range(DEPTH):
        need_ctx = i < DEPTH - 1
        m_lat = (act_c @ ada_w[i] + ada_b[i])[:, None, :]
        m_ctx = act_cc @ ada_w[i] + ada_b[i]
        sh1, sc1, g1, sh2, sc2, g2 = jnp.split(m_lat, 6, axis=-1)
        csh1, csc1, cg1, csh2, csc2, cg2 = jnp.split(m_ctx, 6, axis=-1)
        h_lat = x * (1.0 + sc1) + sh1
        h_ctx = ctx * (1.0 + csc1) + csh1
        j = i // N_MIXERS
        if i % N_MIXERS == 0:
            y_lat, y_ctx = ssd_mixer(h_lat, h_ctx, ssd_w_in[j], ssd_conv_w[j], ssd_conv_b[j],
                                     ssd_dt_bias[j], ssd_a_log[j], ssd_d[j], ssd_norm_w[j],
                                     ssd_w_out[j], need_ctx)
        else:
            y_lat, y_ctx = window_gqa_mixer(h_lat, h_ctx, attn_w_qkv[j], attn_b_qkv[j], attn_sinks[j],
                                            attn_w_o[j], attn_b_o[j], cos, sin, need_ctx)
        x = layer_norm(ALPHA * x + g1 * y_lat, ln_g[i, 0], ln_b[i, 0])
        tokens = (x * (1.0 + sc2) + sh2).reshape(n_lat_tok, D_MODEL)
        if need_ctx:
            ctx = layer_norm(ALPHA * ctx + cg1 * y_ctx, ln_g[i, 0], ln_b[i, 0])
            tokens = jnp.concatenate(
                [tokens, (ctx * (1.0 + csc2) + csh2).reshape(-1, D_MODEL)], axis=0)
        f = moe(tokens, router_w[i], router_b[i], moe_w1[i], moe_b1[i], moe_w2[i], moe_b2[i])
        x = layer_norm(ALPHA * x + g2 * f[:n_lat_tok].reshape(x.shape), ln_g[i, 1], ln_b[i, 1])
        if need_ctx:
            ctx = layer_norm(ALPHA * ctx + cg2 * f[n_lat_tok:].reshape(ctx.shape), ln_g[i, 1], ln_b[i, 1])
    return x
```

```python
import contextlib
import numpy as np
import ml_dtypes
import concourse.bass as bass
import concourse.mybir as mybir
from concourse.bass_utils import run_bass_kernel_spmd

F32 = mybir.dt.float32
BF16 = mybir.dt.bfloat16
AF = mybir.ActivationFunctionType
ALU = mybir.AluOpType
AX = mybir.AxisListType

D = 1024
NB = 2
SEQ = 8192
CTX = 256
DEPTH = 2
ALPHA = (2.0 * DEPTH) ** 0.25
LN_EPS = 1e-5
RMS_EPS = 1e-5
D_INNER = 2048
NEG = -30000.0
SEM_LIMIT = 2000


class T:
    __slots__ = ("w", "r", "name", "dram", "q")
    DRAM_NAMES = ("yfD", "gD", "gsc", "out", "dbg")

    def __init__(self, name=""):
        self.w = None
        self.r = []
        self.name = name
        self.dram = name.startswith(T.DRAM_NAMES)
        self.q = None


class Q:
    def __init__(self, S, name, step):
        self.S = S
        self.name = name
        self.step = step
        self.n = 0
        self._new()

    def _new(self):
        self.sem = self.S.es.enter_context(self.S.nc.semaphore("s_%s_%d" % (self.name, self.n)))
        self.S.allsems.append(self.sem)
        self.sid = len(self.S.allsems)
        self.n += 1
        self.val = 0

    def bump(self):
        if self.val + self.step > SEM_LIMIT:
            self._new()
        self.val += self.step
        return (self.sem, self.val, self, self.sid)


class Eng:
    def __init__(self, S, name, h, compute=True):
        self.name = name
        self.h = h
        self.seen = {}
        self.q = Q(S, name, 1) if compute else None


class Sched:
    def __init__(self, nc, es, safe_same=True):
        self.nc = nc
        self.allsems = []
        self.es = es
        self.safe_same = safe_same
        self.pe = Eng(self, "pe", nc.tensor)
        self.act = Eng(self, "act", nc.scalar)
        self.dve = Eng(self, "dve", nc.vector)
        self.pool = Eng(self, "pool", nc.gpsimd)
        self.sp = Eng(self, "sp", nc.sync, compute=False)
        self.streams = {}
        self.nops = 0

    def stream(self, name):
        if name not in self.streams:
            self.streams[name] = Q(self, "d" + name, 16)
        return self.streams[name]

    def _waits(self, E, reads, writes):
        deps = []
        for t in reads:
            if t.w is not None:
                deps.append(t.w)
        for t in writes:
            if t.w is not None:
                deps.append(t.w)
            deps.extend(t.r)
        seen = E.seen
        for (sem, val, src, sid) in deps:
            if src is E.q and (E is self.pe or not self.safe_same):
                continue
            k = sid
            if seen.get(k, 0) < val:
                E.h.wait_ge(sem, val)
                seen[k] = val

    def _update(self, tok, reads, writes):
        for t in reads:
            t.r.append(tok)
            if len(t.r) > 6:
                last = {}
                for x in t.r:
                    last[x[3]] = x
                t.r = list(last.values())
        for t in writes:
            t.w = tok
            t.r = []

    def op(self, E, reads, writes, emit):
        self._waits(E, reads, writes)
        inst = emit()
        tok = E.q.bump()
        inst.then_inc(tok[0], 1)
        self._update(tok, reads, writes)
        self.nops += 1
        return inst

    def dma(self, E, sname, out, in_, reads, writes):
        self._waits(E, reads, writes)
        anchor = None
        for t in list(writes) + list(reads):
            if not t.dram:
                anchor = t
                break
        if anchor is None:
            q = self.stream(sname)
        else:
            if anchor.q is None:
                self.ndmaq = getattr(self, "ndmaq", 0) + 1
                anchor.q = Q(self, "t%d" % self.ndmaq, 16)
            q = anchor.q
        tok = q.bump()
        E.h.dma_start(out=out, in_=in_).then_inc(tok[0], 16)
        self._update(tok, reads, writes)
        if sname in ("out", "g", "dbg"):
            if not hasattr(self, "out_toks"):
                self.out_toks = []
            self.out_toks.append(tok)

    def wait_all(self, E, tiles):
        self._waits(E, tiles, [])

    def dump(self, name, ap, t, dt=F32):
        if not hasattr(self, "dumps"):
            self.dumps = []
        d = self.nc.dram_tensor("dbg_" + name, list(ap.shape), dt, kind="ExternalOutput").ap()
        dT = T("dbg")
        self.dma(self.sp, "dbg", d, ap, [t], [dT])
        self.dumps.append(dT)


class Ring:
    def __init__(self, items):
        self.items = items
        self.i = -1

    def next(self):
        self.i = (self.i + 1) % len(self.items)
        return self.items[self.i]


def mk(S, name, shape, dt, n=1, es=None):
    items = []
    es = es or S.es
    for i in range(n):
        t = es.enter_context(S.nc.sbuf_tensor("%s_%d" % (name, i), list(shape), dt))
        items.append((t, T(name)))
    return Ring(items) if n > 1 else items[0]


def mkp(S, name):
    t = S.es.enter_context(S.nc.psum_tensor(name, [128, 512], F32))
    return (t, T(name))


def bc(ap, shape):
    return ap.to_broadcast(list(shape))


def emit_ada(S, es, adaw_dram, adab_dram, cT_dram, ncols, mod, modT, pbank, pbT, tag):
    nc = S.nc
    wring = mk(S, "adawt" + tag, [128, 8, 1024], F32, n=2, es=es)
    (cin, cinT) = mk(S, "cin" + tag, [128, 8, 2], F32, es=es)
    (actc, actcT) = mk(S, "actc" + tag, [128, 8, 2], F32, es=es)
    (adab, adabT) = mk(S, "adab" + tag, [128, ncols // 128], F32, es=es)
    S.dma(S.sp, "misc", cin[:], cT_dram, [], [cinT])
    S.dma(S.sp, "misc", adab[:], adab_dram, [], [adabT])
    S.op(S.act, [cinT], [actcT], lambda: nc.scalar.activation(out=actc[:], in_=cin[:], func=AF.Silu))
    nn = ncols // 128
    PIECE = 1024
    for p0 in range(0, ncols, PIECE):
        (wt, wtT) = wring.next()
        S.dma(S.sp, "adaw", wt[:], adaw_dram[:, p0:p0 + PIECE].rearrange("(c p) n -> p c n", p=128), [], [wtT])
        n0 = p0 // 128

        def mm():
            last = None
            for j in range(PIECE // 128):
                for c in range(8):
                    last = nc.tensor.matmul(pbank[:, (n0 + j) * 2:(n0 + j) * 2 + 2], wt[:, c, j * 128:(j + 1) * 128],
                                            actc[:, c, :], start=(c == 0), stop=(c == 7), skip_group_check=True)
            return last
        S.op(S.pe, [wtT, actcT], [pbT], mm)
    S.op(S.dve, [pbT, adabT], [modT],
         lambda: nc.vector.tensor_tensor(out=mod[:, 0:nn, :], in0=pbank[:, 0:nn * 2].rearrange("p (n v) -> p n v", v=2),
                                         in1=bc(adab[:, 0:nn].unsqueeze(2), [128, nn, 2]), op=ALU.add))


NCH_CTX = CTX // 128
NCH_LAT = SEQ // 128
NCH = NCH_CTX + NCH_LAT


def build_A(n_lat_chunks=NCH_LAT, dbg=None):
    nc = bass.Bass("TRN2", target_bir_lowering=False)
    nlat = n_lat_chunks * 128
    nch = NCH_CTX + n_lat_chunks
    dr = lambda name, shape, dt=F32: nc.dram_tensor(name, list(shape), dt, kind="ExternalInput").ap()
    latT = dr("latT", [D, nlat + 2])
    ctxT = dr("ctxT", [D, CTX + 2])
    w_xbc = dr("w_xbc", [D, 768])
    w_z = dr("w_z", [D, 512])
    w_dt = dr("w_dt", [D, 16])
    convw = dr("convw", [128, 6, 3])
    convb = dr("convb", [128, 6])
    dtb = dr("dtb", [1, 16])
    alog = dr("alog", [1, 16])
    dskip = dr("dskip", [1, 8])
    normw = dr("normw", [1, 512])
    cT = dr("cT", [128, 8, 2])
    adaw = dr("adaw", [D, 2048])
    adab = dr("adab", [128, 16])
    tri_d = dr("tri", [2, 128, 128])
    mneg_d = dr("mneg", [2, 128, 128])
    G = nc.dram_tensor("G", [nch * 128, 512], BF16, kind="ExternalOutput").ap()
    Yf = nc.dram_tensor("Yf", [nch, 128, 512], F32, kind="Internal").ap()

    with contextlib.ExitStack() as es:
        S = Sched(nc, es)
        (wx, wxT) = mk(S, "wx", [128, 8, 768], BF16)
        (wz, wzT) = mk(S, "wz", [128, 8, 512], BF16)
        (wd, wdT) = mk(S, "wd", [128, 8, 16], BF16)
        S.dma(S.pool, "w", wx[:], w_xbc.rearrange("(c p) n -> p c n", p=128), [], [wxT])
        S.dma(S.pool, "w", wz[:], w_z.rearrange("(c p) n -> p c n", p=128), [], [wzT])
        S.dma(S.pool, "w", wd[:], w_dt.rearrange("(c p) n -> p c n", p=128), [], [wdT])
        (cw, cwT) = mk(S, "cw", [128, 6, 3], F32)
        (cb, cbT_) = mk(S, "cb", [128, 6], F32)
        S.dma(S.sp, "misc", cw[:], convw, [], [cwT])
        S.dma(S.sp, "misc", cb[:], convb, [], [cbT_])
        (dtbt, dtbT) = mk(S, "dtbt", [128, 16], F32)
        (abc, abcT) = mk(S, "abc", [128, 16], F32)
        (dsk, dskT) = mk(S, "dsk", [128, 8], F32)
        (nw, nwT) = mk(S, "nw", [128, 512], F32)
        S.dma(S.sp, "misc", dtbt[:], dtb.partition_broadcast(128), [], [dtbT])
        S.dma(S.sp, "misc", abc[:], alog.partition_broadcast(128), [], [abcT])
        S.dma(S.sp, "misc", dsk[:], dskip.partition_broadcast(128), [], [dskT])
        S.dma(S.sp, "misc", nw[:], normw.partition_broadcast(128), [], [nwT])
        S.op(S.act, [abcT], [abcT], lambda: nc.scalar.activation(out=abc[:], in_=abc[:], func=AF.Exp))
        S.op(S.dve, [abcT], [abcT], lambda: nc.vector.tensor_scalar(out=abc[:], in0=abc[:], scalar1=-1.0, scalar2=None, op0=ALU.mult))
        (tri, triT) = mk(S, "tri", [128, 2, 128], F32)
        (mneg, mnegT) = mk(S, "mneg", [128, 2, 128], F32)
        S.dma(S.sp, "misc", tri[:], tri_d.rearrange("k p n -> p k n"), [], [triT])
        S.dma(S.sp, "misc", mneg[:], mneg_d.rearrange("k p n -> p k n"), [], [mnegT])
        (ones, onesT) = mk(S, "ones", [128, 128], F32)
        S.op(S.pool, [], [onesT], lambda: nc.gpsimd.memset(ones[:], 1.0))
        (idb, idbT) = mk(S, "idb", [128, 128], BF16)
        S.op(S.pool, [], [idbT], lambda: nc.gpsimd.memset(idb[:], 0.0))
        S.op(S.pool, [idbT], [idbT], lambda: nc.gpsimd.affine_select(out=idb[:], in_=idb[:], pattern=[[-1, 128]], compare_op=ALU.not_equal,
                                                                   fill=1.0, base=0, channel_multiplier=1))
        (dtile, dtileT) = mk(S, "dtile", [128, 8, 64], F32)
        S.op(S.dve, [dskT], [dtileT], lambda: nc.vector.tensor_copy(dtile[:], bc(dsk[:].unsqueeze(2), [128, 8, 64])))

        pxbc0 = mkp(S, "pxbc0")
        pxbc1 = mkp(S, "pxbc1")
        pz = mkp(S, "pz")
        pmisc = mkp(S, "pmisc")
        pcs0 = mkp(S, "pcs0")
        pcs1 = mkp(S, "pcs1")
        pY = mkp(S, "pY")
        pst = mkp(S, "pst")

        (mod, modT) = mk(S, "mod", [128, 16, 2], F32)
        with contextlib.ExitStack() as es2:
            emit_ada(S, es2, adaw, adab, cT, 2048, mod, modT, pz[0], pz[1], "A")
            S.wait_all(S.sp, [modT])
            S.wait_all(S.pool, [modT])
            S.wait_all(S.act, [modT])
            S.wait_all(S.dve, [modT])
            S.wait_all(S.pe, [modT])
        (msc, mscT) = mk(S, "msc", [128, 8, 2], F32)
        S.op(S.dve, [modT], [mscT], lambda: nc.vector.tensor_scalar(out=msc[:], in0=mod[:, 8:16, :], scalar1=1.0, scalar2=None, op0=ALU.add))

        xf_r = mk(S, "xf", [128, 8, 130], F32, n=2)
        hT_r = mk(S, "hT", [128, 8, 130], BF16, n=2)
        acc_r = mk(S, "acc", [128, 6, 128], F32, n=2)
        xbcT_r = mk(S, "xbcT", [128, 6, 128], BF16, n=2)
        tok_r = mk(S, "tok", [128, 640], BF16, n=2)
        cbs_r = mk(S, "cbs", [128, 128], F32, n=2)
        dtv_r = mk(S, "dtv", [128, 16], F32, n=2)
        dt1_r = mk(S, "dt1", [128, 16], F32, n=2)
        dt2_r = mk(S, "dt2", [128, 16], F32, n=2)
        dtA_r = mk(S, "dtA", [128, 16], F32, n=2)
        X_r = mk(S, "X", [128, 8, 128], F32, n=2)
        cs_r = mk(S, "cs", [128, 8], F32, n=2)
        E1_r = mk(S, "E1", [128, 8, 128], F32, n=2)
        Dm_r = mk(S, "Dm", [128, 8, 128], F32, n=2)
        MT_r = mk(S, "MT", [128, 8, 128], BF16, n=2)
        CpT_r = mk(S, "CpT", [128, 8, 128], BF16, n=2)
        dte_r = mk(S, "dte", [128, 8], F32, n=2)
        xdt_r = mk(S, "xdt", [128, 8, 64], BF16, n=2)
        xdtd_r = mk(S, "xdtd", [128, 8, 64], BF16, n=2)
        (St, StT) = mk(S, "St", [128, 8, 64], F32)
        (Sb, SbT) = mk(S, "Sb", [128, 8, 64], BF16)
        ysb_r = mk(S, "ysb", [128, 512], F32, n=2)
        yfl_r = mk(S, "yfl", [128, 512], F32, n=2)
        sz_r = mk(S, "sz", [128, 512], F32, n=2)
        gz_r = mk(S, "gz", [128, 512], F32, n=2)
        sq_r = mk(S, "sq", [128, 512], F32, n=2)
        ss_r = mk(S, "ss", [128, 1], F32, n=2)
        go_r = mk(S, "go", [128, 512], BF16, n=2)

        def chunk(ci, d):
            is_ctx = ci < NCH_CTX
            if is_ctx:
                src = ctxT
                t0 = ci * 128
                first = ci == 0
                last = ci == NCH_CTX - 1
                v = 1
            else:
                src = latT
                t0 = (ci - NCH_CTX) * 128
                first = ci == NCH_CTX
                last = ci == nch - 1
                v = 0
            END = 127 if d == 0 else 0
            (xf, xfT) = xf_r.next()
            (hT, hTT) = hT_r.next()
            S.dma(S.sp, "ld", xf[:], src[:, t0:t0 + 130].rearrange("(c p) t -> p c t", p=128), [], [xfT])

            def modl():
                last_i = None
                for c in range(8):
                    last_i = nc.scalar.activation(out=hT[:, c, :], in_=xf[:, c, :], func=AF.Identity,
                                                  bias=mod[:, c, v:v + 1], scale=msc[:, c, v:v + 1])
                return last_i
            S.op(S.act, [xfT, modT, mscT], [hTT], modl)
            if first:
                S.op(S.pool, [], [hTT], lambda: nc.gpsimd.memset(hT[:, :, 0:1], 0.0))
            if last:
                S.op(S.pool, [], [hTT], lambda: nc.gpsimd.memset(hT[:, :, 129:130], 0.0))
            for half, pb in enumerate((pxbc0, pxbc1)):
                def mm(half=half, pb=pb):
                    li = None
                    for jj in range(3):
                        j = half * 3 + jj
                        for c in range(8):
                            li = nc.tensor.matmul(pb[0][:, jj * 130:(jj + 1) * 130], wx[:, c, j * 128:(j + 1) * 128], hT[:, c, :],
                                                  start=(c == 0), stop=(c == 7), skip_group_check=True)
                    return li
                S.op(S.pe, [wxT, hTT], [pb[1]], mm)

            def mmdt():
                li = None
                for c in range(8):
                    li = nc.tensor.matmul(pmisc[0][:, 0:16], hT[:, c, 1:129], wd[:, c, :], start=(c == 0), stop=(c == 7), skip_group_check=True)
                return li
            S.op(S.pe, [wdT, hTT], [pmisc[1]], mmdt)
            if d == 1:
                def mmz():
                    li = None
                    for c in range(8):
                        li = nc.tensor.matmul(pz[0][:, :], hT[:, c, 1:129], wz[:, c, :], start=(c == 0), stop=(c == 7), skip_group_check=True)
                    return li
                S.op(S.pe, [wzT, hTT], [pz[1]], mmz)
            (acc, accT) = acc_r.next()
            (xbcT, xbcTT) = xbcT_r.next()
            for half, pb in enumerate((pxbc0, pxbc1)):
                for k in range(3):
                    def cv(half=half, pb=pb, k=k):
                        li = None
                        for jj in range(3):
                            j = half * 3 + jj
                            o = jj * 130 + k
                            if k == 0:
                                li = nc.vector.tensor_scalar(out=acc[:, j, :], in0=pb[0][:, o:o + 128], scalar1=cw[:, j, 0:1], scalar2=None, op0=ALU.mult)
                            else:
                                li = nc.vector.scalar_tensor_tensor(out=acc[:, j, :], in0=pb[0][:, o:o + 128], scalar=cw[:, j, k:k + 1], in1=acc[:, j, :],
                                                                    op0=ALU.mult, op1=ALU.add)
                        return li
                    S.op(S.dve, [pb[1], cwT] + ([accT] if k else []), [accT], cv)

            def sl():
                li = None
                for j in range(6):
                    li = nc.scalar.activation(out=xbcT[:, j, :], in_=acc[:, j, :], func=AF.Silu, bias=cb[:, j:j + 1], scale=1.0)
                return li
            S.op(S.act, [accT, cbT_], [xbcTT], sl)
            (dtv, dtvT) = dtv_r.next()
            (dtA, dtAT) = dtA_r.next()
            (dt1, dt1T) = dt1_r.next()
            (dt2, dt2T) = dt2_r.next()
            S.op(S.dve, [pmisc[1], dtbT], [dt1T], lambda: nc.vector.tensor_tensor(out=dt1[:], in0=pmisc[0][:, 0:16], in1=dtbt[:], op=ALU.add))
            S.op(S.act, [dt1T], [dt2T], lambda: nc.scalar.activation(out=dt2[:], in_=dt1[:], func=AF.Exp))
            S.op(S.act, [dt2T], [dtvT], lambda: nc.scalar.activation(out=dtv[:], in_=dt2[:], func=AF.Ln, bias=1.0, scale=1.0))
            S.op(S.dve, [dtvT, abcT], [dtAT], lambda: nc.vector.tensor_tensor(out=dtA[:], in0=dtv[:], in1=abc[:], op=ALU.mult))
            ptr = pmisc[0][:, 64:384].bitcast(BF16)

            def tr():
                li = None
                for j in range(5):
                    li = nc.tensor.transpose(ptr[:, j * 128:(j + 1) * 128], xbcT[:, j, :], idb[:])
                return li
            S.op(S.pe, [xbcTT, idbT], [pmisc[1]], tr)
            (tok, tokT) = tok_r.next()
            S.op(S.act, [pmisc[1]], [tokT], lambda: nc.scalar.copy(out=tok[:], in_=ptr))
            S.op(S.pe, [xbcTT], [pmisc[1]], lambda: nc.tensor.matmul(pmisc[0][:, 384:512], xbcT[:, 4, :], xbcT[:, 5, :], start=True, stop=True,
                                                                      skip_group_check=True))
            (cbs, cbsT) = cbs_r.next()
            S.op(S.act, [pmisc[1]], [cbsT], lambda: nc.scalar.copy(out=cbs[:], in_=pmisc[0][:, 384:512]))
            (X, XT) = X_r.next()
            dsl = slice(d * 8, d * 8 + 8)
            S.op(S.pool, [triT, dtAT], [XT], lambda: nc.gpsimd.tensor_tensor(out=X[:], in0=bc(tri[:, d, :].unsqueeze(1), [128, 8, 128]),
                                                                            in1=bc(dtA[:, dsl].unsqueeze(2), [128, 8, 128]), op=ALU.mult))
            Xf = X[:].rearrange("p h l -> p (h l)")
            S.op(S.pe, [onesT, XT], [pcs0[1]], lambda: nc.tensor.matmul(pcs0[0][:, :], ones[:], Xf[:, 0:512], start=True, stop=True, skip_group_check=True))
            S.op(S.pe, [onesT, XT], [pcs1[1]], lambda: nc.tensor.matmul(pcs1[0][:, :], ones[:], Xf[:, 512:1024], start=True, stop=True, skip_group_check=True))
            S.op(S.pe, [triT, dtAT], [pmisc[1]], lambda: nc.tensor.matmul(pmisc[0][:, 16:24], tri[:, d, :], dtA[:, dsl], start=True, stop=True,
                                                                          skip_group_check=True))
            (cs, csT) = cs_r.next()
            S.op(S.dve, [pmisc[1]], [csT], lambda: nc.vector.tensor_copy(cs[:], pmisc[0][:, 16:24]))
            (E1, E1T) = E1_r.next()
            (Dm, DmT) = Dm_r.next()
            (dte, dteT) = dte_r.next()
            for hh, pc in enumerate((pcs0, pcs1)):
                hs = slice(hh * 4, hh * 4 + 4)
                pv = pc[0][:, :].rearrange("p (h l) -> p h l", l=128)
                S.op(S.act, [pc[1]], [E1T], lambda pv=pv, hs=hs: nc.scalar.activation(out=E1[:, hs, :], in_=pv, func=AF.Exp))
                S.op(S.dve, [pc[1], csT], [DmT], lambda pv=pv, hs=hs: nc.vector.tensor_tensor(out=Dm[:, hs, :], in0=pv,
                                                                                              in1=bc(cs[:, hs].unsqueeze(2), [128, 4, 128]), op=ALU.subtract))
                S.op(S.dve, [pc[1], csT], [dteT], lambda pv=pv, hs=hs: nc.vector.tensor_tensor(out=dte[:, hs], in0=pv[:, :, END], in1=cs[:, hs], op=ALU.subtract))
            S.op(S.dve, [DmT, mnegT], [DmT], lambda: nc.vector.tensor_tensor(out=Dm[:], in0=Dm[:], in1=bc(mneg[:, d, :].unsqueeze(1), [128, 8, 128]), op=ALU.min))
            S.op(S.act, [DmT], [DmT], lambda: nc.scalar.activation(out=Dm[:], in_=Dm[:], func=AF.Exp))
            S.op(S.act, [dteT], [dteT], lambda: nc.scalar.activation(out=dte[:], in_=dte[:], func=AF.Exp))
            (MT, MTT) = MT_r.next()
            S.op(S.dve, [DmT, cbsT], [MTT], lambda: nc.vector.tensor_tensor(out=MT[:], in0=Dm[:], in1=bc(cbs[:].unsqueeze(1), [128, 8, 128]), op=ALU.mult))
            (CpT, CpTT) = CpT_r.next()
            S.op(S.pool, [E1T, xbcTT], [CpTT], lambda: nc.gpsimd.tensor_tensor(out=CpT[:], in0=E1[:], in1=bc(xbcT[:, 5, :].unsqueeze(1), [128, 8, 128]), op=ALU.mult))
            (xdt, xdtT) = xdt_r.next()
            (xdtd, xdtdT) = xdtd_r.next()
            xs3 = tok[:, 0:512].rearrange("p (h e) -> p h e", e=64)
            S.op(S.pool, [tokT, dtvT], [xdtT], lambda: nc.gpsimd.tensor_tensor(out=xdt[:], in0=xs3, in1=bc(dtv[:, dsl].unsqueeze(2), [128, 8, 64]), op=ALU.mult))
            S.op(S.pool, [xdtT, dteT], [xdtdT], lambda: nc.gpsimd.tensor_tensor(out=xdtd[:], in0=xdt[:], in1=bc(dte[:].unsqueeze(2), [128, 8, 64]), op=ALU.mult))

            def mmy():
                li = None
                for h in range(8):
                    nc.tensor.matmul(pY[0][:, h * 64:(h + 1) * 64], MT[:, h, :], xdt[:, h, :], start=(h == 0), stop=False, skip_group_check=True)
                    li = nc.tensor.matmul(pY[0][:, h * 64:(h + 1) * 64], CpT[:, h, :], Sb[:, h, :], start=False, stop=True, skip_group_check=True)
                return li
            S.op(S.pe, [MTT, xdtT, CpTT, SbT], [pY[1]], mmy)
            S.op(S.pe, [tokT, xdtdT], [pst[1]], lambda: nc.tensor.matmul(pst[0][:, :], tok[:, 512:640], xdtd[:].rearrange("p h e -> p (h e)"),
                                                                         start=True, stop=True, skip_group_check=True))
            S.op(S.dve, [StT, E1T], [StT], lambda: nc.vector.tensor_tensor(out=St[:], in0=St[:], in1=bc(E1[:, :, END:END + 1], [128, 8, 64]), op=ALU.mult))
            S.op(S.dve, [StT, pst[1]], [StT], lambda: nc.vector.tensor_tensor(out=St[:], in0=St[:], in1=pst[0][:, :].rearrange("p (h e) -> p h e", e=64), op=ALU.add))
            S.op(S.pool, [StT], [SbT], lambda: nc.gpsimd.tensor_copy(Sb[:], St[:]))
            if dbg is not None and (ci, d) == dbg:
                S.dump("hT", hT[:], hTT, BF16)
                S.dump("dt1", dt1[:], dt1T)
                S.dump("dt2", dt2[:], dt2T)
                S.dump("dtbt", dtbt[:], dtbT)
                S.dump("abc", abc[:], abcT)
                S.dump("acc", acc[:], accT)
                S.dump("xbcT", xbcT[:], xbcTT, BF16)
                S.dump("tok", tok[:], tokT, BF16)
                S.dump("dtv", dtv[:], dtvT)
                S.dump("dtA", dtA[:], dtAT)
                S.dump("cs", cs[:], csT)
                S.dump("E1", E1[:], E1T)
                S.dump("Dm", Dm[:], DmT)
                S.dump("cbs", cbs[:], cbsT)
                S.dump("MT", MT[:], MTT, BF16)
                S.dump("CpT", CpT[:], CpTT, BF16)
                S.dump("xdt", xdt[:], xdtT, BF16)
                S.dump("xdtd", xdtd[:], xdtdT, BF16)
                S.dump("dte", dte[:], dteT)
                S.dump("St", St[:], StT)
                S.dump("mod", mod[:], modT)
            if d == 0:
                (ysb, ysbT) = ysb_r.next()
                S.op(S.act, [pY[1]], [ysbT], lambda: nc.scalar.copy(out=ysb[:], in_=pY[0][:, :]))
                S.dma(S.sp, "yf", Yf[ci], ysb[:], [ysbT], [yfD[ci]])
            else:
                (yfl, yflT) = yfl_r.next()
                S.dma(S.sp, "yfl", yfl[:], Yf[ci], [yfD[ci]], [yflT])
                (gz, gzT) = gz_r.next()
                (sz, szT) = sz_r.next()
                (sq, sqT) = sq_r.next()
                (ss, ssT) = ss_r.next()
                (go, goT) = go_r.next()
                S.op(S.dve, [pY[1], yflT], [gzT], lambda: nc.vector.tensor_tensor(out=gz[:], in0=pY[0][:, :], in1=yfl[:], op=ALU.add))
                S.op(S.pool, [tokT, dtileT], [sqT], lambda: nc.gpsimd.tensor_tensor(out=sq[:], in0=tok[:, 0:512], in1=dtile[:].rearrange("p h e -> p (h e)"), op=ALU.mult))
                S.op(S.dve, [gzT, sqT], [gzT], lambda: nc.vector.tensor_tensor(out=gz[:], in0=gz[:], in1=sq[:], op=ALU.add))
                S.op(S.act, [pz[1]], [szT], lambda: nc.scalar.activation(out=sz[:], in_=pz[0][:, :], func=AF.Silu))
                S.op(S.dve, [gzT, szT], [gzT], lambda: nc.vector.tensor_tensor(out=gz[:], in0=gz[:], in1=sz[:], op=ALU.mult))
                S.op(S.act, [gzT], [sqT, ssT], lambda: nc.scalar.activation(out=sq[:], in_=gz[:], func=AF.Square, accum_out=ss[:]))
                S.op(S.dve, [ssT], [ssT], lambda: nc.vector.tensor_scalar(out=ss[:], in0=ss[:], scalar1=1.0 / 512.0, scalar2=RMS_EPS, op0=ALU.mult, op1=ALU.add))
                S.op(S.act, [ssT], [ssT], lambda: nc.scalar.activation(out=ss[:], in_=ss[:], func=AF.Sqrt))
                S.op(S.dve, [ssT], [ssT], lambda: nc.vector.reciprocal(out=ss[:], in_=ss[:]))
                S.op(S.dve, [gzT, ssT, nwT], [goT], lambda: nc.vector.scalar_tensor_tensor(out=go[:], in0=gz[:], scalar=ss[:, 0:1], in1=nw[:], op0=ALU.mult, op1=ALU.mult))
                S.dma(S.sp, "g", G[ci * 128:(ci + 1) * 128, :], go[:], [goT], [gD])

        yfD = [T("yfD%d" % i) for i in range(nch)]
        gD = T("gD")
        S.op(S.pool, [], [StT], lambda: nc.gpsimd.memset(St[:], 0.0))
        S.op(S.pool, [], [SbT], lambda: nc.gpsimd.memset(Sb[:], 0.0))
        for ci in range(nch):
            chunk(ci, 0)
        S.op(S.pool, [StT], [StT], lambda: nc.gpsimd.memset(St[:], 0.0))
        S.op(S.pool, [SbT], [SbT], lambda: nc.gpsimd.memset(Sb[:], 0.0))
        order = list(range(NCH_CTX - 1, -1, -1)) + list(range(nch - 1, NCH_CTX - 1, -1))
        for ci in order:
            chunk(ci, 1)
        S.wait_all(S.sp, [gD] + getattr(S, "dumps", []))
        for tok in S.out_toks:
            nc.sync.wait_ge(tok[0], tok[1])
        for E in (S.pe, S.act, S.dve, S.pool):
            S.wait_all(E, [gD])
    return nc


def host_inputs_A(inp, n_lat_chunks=NCH_LAT):
    nlat = n_lat_chunks * 128
    maps = []
    w_in = inp["ssd_w_in"][0]
    conv_w = inp["ssd_conv_w"][0]
    conv_b = inp["ssd_conv_b"][0]
    r = np.arange(128)
    tri_f = (r[:, None] <= r[None, :]).astype(np.float32)
    tri_b = (r[:, None] >= r[None, :]).astype(np.float32)
    tri = np.stack([tri_f, tri_b])
    mneg = np.where(tri > 0, 0.0, NEG).astype(np.float32)
    for core in range(8):
        b, g = core // 4, core % 4
        latT = np.zeros((D, nlat + 2), np.float32)
        latT[:, 1:nlat + 1] = inp["x"][b, :nlat].T
        ctxT = np.zeros((D, CTX + 2), np.float32)
        ctxT[:, 1:CTX + 1] = inp["ctx"][b].T
        xc = np.arange(D_INNER + g * 512, D_INNER + (g + 1) * 512)
        bcol = np.arange(2 * D_INNER + g * 128, 2 * D_INNER + (g + 1) * 128)
        ccol = np.arange(2 * D_INNER + 512 + g * 128, 2 * D_INNER + 512 + (g + 1) * 128)
        cols = np.concatenate([xc, bcol, ccol])
        ch = cols - D_INNER
        dtc = np.concatenate([np.arange(5120 + g * 8, 5120 + g * 8 + 8), np.arange(5152 + g * 8, 5152 + g * 8 + 8)])
        cT = np.stack([inp["c"][b].reshape(8, 128).T, inp["c_ctx"].reshape(8, 128).T], axis=-1)
        m = {
            "latT": latT, "ctxT": ctxT,
            "w_xbc": np.ascontiguousarray(w_in[:, cols]),
            "w_z": np.ascontiguousarray(w_in[:, g * 512:(g + 1) * 512]),
            "w_dt": np.ascontiguousarray(w_in[:, dtc]),
            "convw": np.ascontiguousarray(conv_w[:, ch].T.reshape(6, 128, 3).transpose(1, 0, 2)),
            "convb": np.ascontiguousarray(conv_b[ch].reshape(6, 128).T),
            "dtb": np.concatenate([inp["ssd_dt_bias"][0, 0, g * 8:(g + 1) * 8], inp["ssd_dt_bias"][0, 1, g * 8:(g + 1) * 8]])[None, :],
            "alog": np.concatenate([inp["ssd_a_log"][0, 0, g * 8:(g + 1) * 8], inp["ssd_a_log"][0, 1, g * 8:(g + 1) * 8]])[None, :],
            "dskip": inp["ssd_d"][0, g * 8:(g + 1) * 8][None, :],
            "normw": inp["ssd_norm_w"][0, g * 512:(g + 1) * 512][None, :],
            "cT": np.ascontiguousarray(cT),
            "adaw": np.ascontiguousarray(inp["ada_w"][0][:, 0:2048]),
            "adab": np.ascontiguousarray(inp["ada_b"][0][0:2048].reshape(16, 128).T),
            "tri": tri, "mneg": mneg,
        }
        maps.append({k: np.ascontiguousarray(v, dtype=np.float32) for k, v in m.items()})
    return maps


def run_A(inp, n_lat_chunks=NCH_LAT, dbg=None):
    nc = build_A(n_lat_chunks, dbg)
    maps = host_inputs_A(inp, n_lat_chunks)
    res = run_bass_kernel_spmd(nc, maps, core_ids=list(range(8)))
    if dbg is not None:
        return res.results
    return [r["G"] for r in res.results]


def emit_ln(S, es, X, Xt, tiles, lnp, lnpT, gi, bi, ones, onesT, epsc, epscT, pS1, pS2, tag):
    nc = S.nc
    sq_r = mk(S, "lnsq" + tag, [128, 512], F32, n=3, es=es)
    mean_r = mk(S, "lnmean" + tag, [128, 512], F32, n=2, es=es)
    msq_r = mk(S, "lnmsq" + tag, [128, 512], F32, n=2, es=es)
    rstd_r = mk(S, "lnrstd" + tag, [128, 512], F32, n=2, es=es)
    for ti, (t0, n, v) in enumerate(tiles):
        xt = Xt[ti]

        def s1():
            li = None
            for m in range(8):
                li = nc.tensor.matmul(pS1[0][:, 0:n], ones[:], X[:, m, t0:t0 + n], start=(m == 0), stop=(m == 7), skip_group_check=True)
            return li
        S.op(S.pe, [xt, onesT], [pS1[1]], s1)
        for m in range(8):
            (sq, sqT) = sq_r.next()
            S.op(S.act, [xt], [sqT], lambda sq=sq, m=m: nc.scalar.activation(out=sq[:, 0:n], in_=X[:, m, t0:t0 + n], func=AF.Square))
            S.op(S.pe, [sqT, onesT], [pS2[1]], lambda sq=sq, m=m: nc.tensor.matmul(pS2[0][:, 0:n], ones[:], sq[:, 0:n], start=(m == 0), stop=(m == 7),
                                                                                   skip_group_check=True))
        (mean, meanT) = mean_r.next()
        (msq, msqT) = msq_r.next()
        (rstd, rstdT) = rstd_r.next()
        S.op(S.dve, [pS1[1]], [meanT], lambda: nc.vector.tensor_scalar(out=mean[:, 0:n], in0=pS1[0][:, 0:n], scalar1=1.0 / D, scalar2=None, op0=ALU.mult))
        S.op(S.pool, [meanT], [msqT], lambda: nc.gpsimd.tensor_tensor(out=msq[:, 0:n], in0=mean[:, 0:n], in1=mean[:, 0:n], op=ALU.mult))
        S.op(S.dve, [pS2[1], msqT], [rstdT], lambda: nc.vector.scalar_tensor_tensor(out=rstd[:, 0:n], in0=pS2[0][:, 0:n], scalar=1.0 / D, in1=msq[:, 0:n],
                                                                                    op0=ALU.mult, op1=ALU.subtract))
        S.op(S.act, [rstdT, epscT], [rstdT], lambda: nc.scalar.activation(out=rstd[:, 0:n], in_=rstd[:, 0:n], func=AF.Sqrt, bias=epsc[:, 0:1], scale=1.0))
        S.op(S.dve, [rstdT], [rstdT], lambda: nc.vector.reciprocal(out=rstd[:, 0:n], in_=rstd[:, 0:n]))
        for m in range(8):
            xs = X[:, m, t0:t0 + n]
            S.op(S.dve, [xt, meanT], [xt], lambda xs=xs: nc.vector.tensor_tensor(out=xs, in0=xs, in1=mean[:, 0:n], op=ALU.subtract))
            S.op(S.pool, [xt, rstdT], [xt], lambda xs=xs: nc.gpsimd.tensor_tensor(out=xs, in0=xs, in1=rstd[:, 0:n], op=ALU.mult))
            S.op(S.act, [xt, lnpT], [xt], lambda xs=xs, m=m: nc.scalar.activation(out=xs, in_=xs, func=AF.Identity, bias=lnp[:, m, bi:bi + 1],
                                                                                 scale=lnp[:, m, gi:gi + 1]))


def emit_moe(S, es, X, Xt, tiles, NT, mod, modT, i_sh, i_sc, i_g, P, dr, experts, identf, identfT, tag, hoff=0):
    nc = S.nc
    (hT, hTT) = mk(S, "hT" + tag, [128, 8, NT], BF16, es=es)
    hTt = [T("hTt") for _ in tiles]
    (msc, mscT) = mk(S, "msc2" + tag, [128, 8, 2], F32, es=es)
    S.op(S.dve, [modT], [mscT], lambda: nc.vector.tensor_scalar(out=msc[:], in0=mod[:, i_sc:i_sc + 8, :], scalar1=1.0, scalar2=None, op0=ALU.add))
    (wr, wrT) = mk(S, "wr" + tag, [128, 8, 32], F32, es=es)
    S.dma(S.sp, "misc", wr[:], dr["wr"].rearrange("(c p) n -> p c n", p=128), [], [wrT])
    (brb, brbT) = mk(S, "brb" + tag, [128, 32], F32, es=es)
    S.dma(S.sp, "misc", brb[:], dr["br"].partition_broadcast(128), [], [brbT])
    (b1, b1T) = mk(S, "b1" + tag, [128, 32, 16], F32, es=es)
    S.dma(S.sp, "misc", b1[:], dr["b1"], [], [b1T])
    S.op(S.dve, [b1T], [b1T], lambda: nc.vector.tensor_scalar(out=b1[:, :, 8:16], in0=b1[:, :, 8:16], scalar1=1.0, scalar2=None, op0=ALU.add))
    (b2, b2T) = mk(S, "b2" + tag, [32, 1024], F32, es=es)
    S.dma(S.sp, "misc", b2[:], dr["b2"], [], [b2T])
    gscT = [T("gsc") for _ in tiles]
    pR = P[6]
    pG = P[7]
    with contextlib.ExitStack() as es2:
        (h32, h32T) = mk(S, "h32" + tag, [128, 8, 512], F32, es=es2)
        lg_r = mk(S, "lg" + tag, [128, 4, 32], F32, n=2, es=es2)
        gt_r = mk(S, "gt" + tag, [128, 4, 32], F32, n=2, es=es2)
        mx_r = mk(S, "mx" + tag, [128, 8], F32, n=2, es=es2)
        ex_r = mk(S, "ex" + tag, [128, 32], F32, n=2, es=es2)
        mk_r = mk(S, "mk" + tag, [128, 32], F32, n=2, es=es2)
        sm_r = mk(S, "sm" + tag, [128, 2], F32, n=2, es=es2)
        gts_r = mk(S, "gts" + tag, [32, 512], F32, n=2, es=es2)
        for ti, (t0, n, v) in enumerate(tiles):
            xt = Xt[ti]
            nb = (n + 127) // 128

            def modl():
                li = None
                for m in range(8):
                    li = nc.scalar.activation(out=h32[:, m, 0:n], in_=X[:, m, t0:t0 + n], func=AF.Identity,
                                              bias=mod[:, i_sh + m, v:v + 1], scale=msc[:, m, v:v + 1])
                return li
            S.op(S.act, [xt, modT, mscT], [h32T], modl)
            S.op(S.pool, [h32T], [hTt[ti]], lambda: nc.gpsimd.tensor_copy(hT[:, :, t0 - hoff:t0 - hoff + n], h32[:, :, 0:n]))

            def rt():
                li = None
                for k in range(nb):
                    kn = min(128, n - k * 128)
                    for m in range(8):
                        li = nc.tensor.matmul(pR[0][0:kn, k * 32:(k + 1) * 32], h32[:, m, k * 128:k * 128 + kn], wr[:, m, :],
                                              start=(m == 0), stop=(m == 7), skip_group_check=True)
                return li
            S.op(S.pe, [h32T, wrT], [pR[1]], rt)
            kn0 = min(128, n)
            (lg, lgT) = lg_r.next()
            (gt, gtT) = gt_r.next()
            S.op(S.dve, [pR[1], brbT], [lgT], lambda: nc.vector.tensor_tensor(out=lg[0:kn0, 0:nb, :], in0=pR[0][0:kn0, 0:nb * 32].rearrange("p (k e) -> p k e", e=32),
                                                                              in1=bc(brb[0:kn0, :].unsqueeze(1), [kn0, nb, 32]), op=ALU.add))
            for k in range(nb):
                (mx, mxT) = mx_r.next()
                (ex, exT) = ex_r.next()
                (mkk, mkT) = mk_r.next()
                (sm, smT) = sm_r.next()
                S.op(S.dve, [lgT], [mxT], lambda: nc.vector.max(out=mx[0:kn0, :], in_=lg[0:kn0, k, :]))
                S.op(S.dve, [mxT], [smT], lambda: nc.vector.tensor_scalar(out=sm[0:kn0, 0:1], in0=mx[0:kn0, 0:1], scalar1=-1.0, scalar2=None, op0=ALU.mult))
                S.op(S.act, [lgT, smT], [exT], lambda: nc.scalar.activation(out=ex[0:kn0, :], in_=lg[0:kn0, k, :], func=AF.Exp, bias=sm[0:kn0, 0:1], scale=1.0))
                S.op(S.dve, [lgT, mxT], [mkT], lambda: nc.vector.tensor_scalar(out=mkk[0:kn0, :], in0=lg[0:kn0, k, :], scalar1=mx[0:kn0, 3:4], scalar2=None, op0=ALU.is_ge))
                S.op(S.dve, [exT, mkT], [exT], lambda: nc.vector.tensor_tensor(out=ex[0:kn0, :], in0=ex[0:kn0, :], in1=mkk[0:kn0, :], op=ALU.mult))
                S.op(S.dve, [exT], [smT], lambda: nc.vector.reduce_sum(out=sm[0:kn0, 1:2], in_=ex[0:kn0, :], axis=AX.X))
                S.op(S.dve, [smT], [smT], lambda: nc.vector.reciprocal(out=sm[0:kn0, 1:2], in_=sm[0:kn0, 1:2]))
                S.op(S.dve, [exT, smT], [gtT], lambda: nc.vector.tensor_scalar(out=gt[0:kn0, k, :], in0=ex[0:kn0, :], scalar1=sm[0:kn0, 1:2], scalar2=None, op0=ALU.mult))

            def trg():
                li = None
                for k in range(nb):
                    li = nc.tensor.transpose(pG[0][0:32, k * 128:k * 128 + kn0], gt[0:kn0, k, :], identf[0:kn0, 0:kn0])
                return li
            S.op(S.pe, [gtT, identfT], [pG[1]], trg)
            (gts, gtsT) = gts_r.next()
            S.op(S.act, [pG[1]], [gtsT], lambda: nc.scalar.copy(out=gts[:, 0:n], in_=pG[0][0:32, 0:n]))
            S.dma(S.sp, "gsc", dr["gsc"][:, t0 - hoff:t0 - hoff + n], gts[:, 0:n], [gtsT], [gscT[ti]])
            S.op(S.act, [xt], [xt], lambda: nc.scalar.activation(out=X[:, :, t0:t0 + n], in_=X[:, :, t0:t0 + n], func=AF.Copy, scale=ALPHA))
            for m in range(8):
                pb = P[m % 2]
                S.op(S.pe, [b2T, gtsT], [pb[1]], lambda pb=pb, m=m: nc.tensor.matmul(pb[0][:, 0:n], b2[:, m * 128:(m + 1) * 128], gts[:, 0:n], start=True, stop=True,
                                                                                     skip_group_check=True))
                S.op(S.dve, [pb[1], modT, xt], [xt], lambda pb=pb, m=m: nc.vector.scalar_tensor_tensor(out=X[:, m, t0:t0 + n], in0=pb[0][:, 0:n], scalar=mod[:, i_g + m, v:v + 1],
                                                                                                       in1=X[:, m, t0:t0 + n], op0=ALU.mult, op1=ALU.add))
        for E in (S.sp, S.pool, S.act, S.dve, S.pe):
            S.wait_all(E, [h32T, lgT, gtT, mxT, exT, mkT, smT, gtsT] + [x[1] for x in gts_r.items] + [x[1] for x in lg_r.items] + [x[1] for x in gt_r.items])
    (w1g, _) = mk(S, "w1g" + tag, [128, 8, 1024], BF16, es=es)
    (w1l, _) = mk(S, "w1l" + tag, [128, 8, 1024], BF16, es=es)
    (w2, _) = mk(S, "w2" + tag, [128, 8, 1024], BF16, es=es)
    w1gT = [T("w1g0"), T("w1g1")]
    w1lT = [T("w1l0"), T("w1l1")]
    w2T = [T("w20"), T("w21")]
    gb_r = mk(S, "gb" + tag, [128, 512], F32, n=3, es=es)
    glu_r = mk(S, "glu" + tag, [128, 512], F32, n=2, es=es)
    sig_r = mk(S, "sig" + tag, [128, 512], F32, n=2, es=es)
    lin_r = mk(S, "lin" + tag, [128, 512], F32, n=2, es=es)
    act_r = mk(S, "act" + tag, [128, 8, 512], BF16, n=2, es=es)
    phg_r = Ring([P[0], P[1]])
    phl_r = Ring([P[2], P[3]])
    py_r = Ring([P[4], P[5]])
    for e in experts:
        for fh in range(2):
            fs = slice(fh * 512, (fh + 1) * 512)
            S.dma(S.pool, "w", w1g[:, :, fs], dr["w1g"][e][:, fs].rearrange("(c p) f -> p c f", p=128), [], [w1gT[fh]])
            S.dma(S.pool, "w", w1l[:, :, fs], dr["w1l"][e][:, fs].rearrange("(c p) f -> p c f", p=128), [], [w1lT[fh]])
        for dh in range(2):
            ds = slice(dh * 512, (dh + 1) * 512)
            S.dma(S.pool, "w", w2[:, :, ds], dr["w2"][e][:, ds].rearrange("(c p) f -> p c f", p=128), [], [w2T[dh]])
        for ti, (t0, n, v) in enumerate(tiles):
            xt = Xt[ti]
            (gb, gbT) = gb_r.next()
            S.dma(S.sp, "gb", gb[:, 0:n], dr["gsc"][e:e + 1, t0 - hoff:t0 - hoff + n].partition_broadcast(128), [gscT[ti]], [gbT])
            (act, actT) = act_r.next()
            for j in range(8):
                phg = phg_r.next()
                phl = phl_r.next()
                fh = j // 4

                def mg(phg=phg, j=j):
                    li = None
                    for c in range(8):
                        li = nc.tensor.matmul(phg[0][:, 0:n], w1g[:, c, j * 128:(j + 1) * 128], hT[:, c, t0 - hoff:t0 - hoff + n], start=(c == 0), stop=(c == 7), skip_group_check=True)
                    return li

                def ml(phl=phl, j=j):
                    li = None
                    for c in range(8):
                        li = nc.tensor.matmul(phl[0][:, 0:n], w1l[:, c, j * 128:(j + 1) * 128], hT[:, c, t0 - hoff:t0 - hoff + n], start=(c == 0), stop=(c == 7), skip_group_check=True)
                    return li
                S.op(S.pe, [w1gT[fh], hTt[ti]], [phg[1]], mg)
                S.op(S.pe, [w1lT[fh], hTt[ti]], [phl[1]], ml)
                (glu, gluT) = glu_r.next()
                (sig, sigT) = sig_r.next()
                (lin, linT) = lin_r.next()
                S.op(S.dve, [phg[1], b1T], [gluT], lambda phg=phg, j=j, glu=glu: nc.vector.tensor_scalar(out=glu[:, 0:n], in0=phg[0][:, 0:n], scalar1=b1[:, e, j:j + 1], scalar2=7.0,
                                                                                                         op0=ALU.add, op1=ALU.min))
                S.op(S.act, [gluT], [sigT], lambda glu=glu, sig=sig: nc.scalar.activation(out=sig[:, 0:n], in_=glu[:, 0:n], func=AF.Sigmoid, scale=1.702))
                S.op(S.dve, [phl[1], b1T], [linT], lambda phl=phl, j=j, lin=lin: nc.vector.tensor_scalar(out=lin[:, 0:n], in0=phl[0][:, 0:n], scalar1=b1[:, e, 8 + j:9 + j], scalar2=8.0,
                                                                                                         op0=ALU.add, op1=ALU.min))
                S.op(S.pool, [gluT, sigT], [gluT], lambda glu=glu, sig=sig: nc.gpsimd.tensor_tensor(out=glu[:, 0:n], in0=glu[:, 0:n], in1=sig[:, 0:n], op=ALU.mult))
                S.op(S.pool, [gluT, gbT], [gluT], lambda glu=glu, gb=gb: nc.gpsimd.tensor_tensor(out=glu[:, 0:n], in0=glu[:, 0:n], in1=gb[:, 0:n], op=ALU.mult))
                S.op(S.dve, [linT, gluT], [actT], lambda lin=lin, glu=glu, j=j, act=act: nc.vector.scalar_tensor_tensor(out=act[:, j, 0:n], in0=lin[:, 0:n], scalar=-6.0, in1=glu[:, 0:n],
                                                                                                                     op0=ALU.max, op1=ALU.mult))
            for m in range(8):
                py = py_r.next()
                dh = m // 4

                def my(py=py, m=m, act=act):
                    li = None
                    for j in range(8):
                        li = nc.tensor.matmul(py[0][:, 0:n], w2[:, j, m * 128:(m + 1) * 128], act[:, j, 0:n], start=(j == 0), stop=(j == 7), skip_group_check=True)
                    return li
                S.op(S.pe, [w2T[dh], actT], [py[1]], my)
                S.op(S.dve, [py[1], modT, xt], [xt], lambda py=py, m=m: nc.vector.scalar_tensor_tensor(out=X[:, m, t0:t0 + n], in0=py[0][:, 0:n], scalar=mod[:, i_g + m, v:v + 1],
                                                                                                       in1=X[:, m, t0:t0 + n], op0=ALU.mult, op1=ALU.add))
    return [hTT] + hTt + w1gT + w1lT + w2T + [x[1] for r in (gb_r, glu_r, sig_r, lin_r, act_r) for x in r.items]


def drain(S, tiles):
    for E in (S.sp, S.pool, S.act, S.dve, S.pe):
        S.wait_all(E, tiles)


def common_consts(S):
    nc = S.nc
    (ones, onesT) = mk(S, "ones", [128, 128], F32)
    S.op(S.pool, [], [onesT], lambda: nc.gpsimd.memset(ones[:], 1.0))
    (identf, identfT) = mk(S, "identf", [128, 128], F32)
    S.op(S.pool, [], [identfT], lambda: nc.gpsimd.memset(identf[:], 0.0))
    S.op(S.pool, [identfT], [identfT], lambda: nc.gpsimd.affine_select(out=identf[:], in_=identf[:], pattern=[[-1, 128]], compare_op=ALU.not_equal,
                                                                       fill=1.0, base=0, channel_multiplier=1))
    (epsc, epscT) = mk(S, "epsc", [128, 1], F32)
    S.op(S.pool, [], [epscT], lambda: nc.gpsimd.memset(epsc[:], LN_EPS))
    return ones, onesT, identf, identfT, epsc, epscT


B1_TILES = [(0, 512, 0), (512, 512, 0), (1024, 512, 0), (1536, 512, 0), (2048, 64, 1)]
B1_NT = 2112


def moe_dram(nc, L, nexp):
    dr = lambda name, shape, dt=F32: nc.dram_tensor(name, list(shape), dt, kind="ExternalInput").ap()
    d = {
        "wr": dr("wr" + L, [D, 32]), "br": dr("br" + L, [1, 32]),
        "w1g": dr("w1g" + L, [nexp, D, 1024]), "w1l": dr("w1l" + L, [nexp, D, 1024]), "w2": dr("w2" + L, [nexp, 1024, D]),
        "b1": dr("b1" + L, [128, 32, 16]), "b2": dr("b2" + L, [32, D]),
    }
    return d


def build_B1(experts=tuple(range(32)), tiles=B1_TILES, NT=B1_NT):
    nc = bass.Bass("TRN2", target_bir_lowering=False)
    dr = lambda name, shape, dt=F32: nc.dram_tensor(name, list(shape), dt, kind="ExternalInput").ap()
    xT = dr("xT", [D, NT])
    gT = dr("gT", [D_INNER, NT], BF16)
    w_out = dr("w_out", [D_INNER, D])
    adaw = dr("adaw", [D, 4096])
    adab = dr("adab", [128, 32])
    cT = dr("cT", [128, 8, 2])
    lnp_d = dr("lnp", [128, 8, 4])
    md = moe_dram(nc, "0", 32)
    md["gsc"] = nc.dram_tensor("gsc", [32, NT], F32, kind="Internal").ap()
    outT = nc.dram_tensor("outT", [D, NT], F32, kind="ExternalOutput").ap()
    with contextlib.ExitStack() as es:
        S = Sched(nc, es)
        ones, onesT, identf, identfT, epsc, epscT = common_consts(S)
        P = [mkp(S, "P%d" % i) for i in range(8)]
        (X, _) = mk(S, "X", [128, 8, NT], F32)
        Xt = [T("Xt%d" % i) for i in range(len(tiles))]
        for ti, (t0, n, v) in enumerate(tiles):
            S.dma(S.sp, "ld", X[:, :, t0:t0 + n], xT[:, t0:t0 + n].rearrange("(c p) t -> p c t", p=128), [], [Xt[ti]])
        (lnp, lnpT) = mk(S, "lnp", [128, 8, 4], F32)
        S.dma(S.sp, "misc", lnp[:], lnp_d, [], [lnpT])
        (mod, modT) = mk(S, "mod", [128, 32, 2], F32)
        with contextlib.ExitStack() as es2:
            emit_ada(S, es2, adaw, adab, cT, 4096, mod, modT, P[7][0], P[7][1], "B1")
            drain(S, [modT])
        with contextlib.ExitStack() as es2:
            (wo, woT) = mk(S, "wo", [128, 16, 1024], BF16, es=es2)
            S.dma(S.pool, "w", wo[:], w_out.rearrange("(k p) n -> p k n", p=128), [], [woT])
            g_r = mk(S, "gTt", [128, 16, 512], BF16, n=2, es=es2)
            py_r = Ring([P[0], P[1]])
            for ti, (t0, n, v) in enumerate(tiles):
                (gt, gtT) = g_r.next()
                S.dma(S.sp, "ld", gt[:, :, 0:n], gT[:, t0:t0 + n].rearrange("(k p) t -> p k t", p=128), [], [gtT])
                S.op(S.act, [Xt[ti]], [Xt[ti]], lambda: nc.scalar.activation(out=X[:, :, t0:t0 + n], in_=X[:, :, t0:t0 + n], func=AF.Copy, scale=ALPHA))
                for m in range(8):
                    py = py_r.next()

                    def mmo(py=py, m=m, gt=gt):
                        li = None
                        for k in range(16):
                            li = nc.tensor.matmul(py[0][:, 0:n], wo[:, k, m * 128:(m + 1) * 128], gt[:, k, 0:n], start=(k == 0), stop=(k == 15), skip_group_check=True)
                        return li
                    S.op(S.pe, [woT, gtT], [py[1]], mmo)
                    S.op(S.dve, [py[1], modT, Xt[ti]], [Xt[ti]], lambda py=py, m=m: nc.vector.scalar_tensor_tensor(
                        out=X[:, m, t0:t0 + n], in0=py[0][:, 0:n], scalar=mod[:, 0 + m, v:v + 1], in1=X[:, m, t0:t0 + n], op0=ALU.mult, op1=ALU.add))
            emit_ln(S, es2, X, Xt, tiles, lnp, lnpT, 0, 1, ones, onesT, epsc, epscT, P[2], P[3], "a")
            drain(S, Xt + [woT])
        with contextlib.ExitStack() as es2:
            used = emit_moe(S, es2, X, Xt, tiles, NT, mod, modT, 8, 16, 24, P, md, experts, identf, identfT, "m0")
            drain(S, Xt + used)
        with contextlib.ExitStack() as es2:
            emit_ln(S, es2, X, Xt, tiles, lnp, lnpT, 2, 3, ones, onesT, epsc, epscT, P[2], P[3], "b")
            oT = T("out")
            for ti, (t0, n, v) in enumerate(tiles):
                S.dma(S.sp, "out", outT[:, t0:t0 + n].rearrange("(c p) t -> p c t", p=128), X[:, :, t0:t0 + n], [Xt[ti]], [oT])
            drain(S, Xt + [oT])
            for tok in S.out_toks:
                nc.sync.wait_ge(tok[0], tok[1])
    return nc


def moe_host(inp, L):
    w1 = inp["moe_w1"][L]
    b1 = inp["moe_b1"][L]
    b1g = b1[:, 0::2].reshape(32, 8, 128).transpose(2, 0, 1)
    b1l = b1[:, 1::2].reshape(32, 8, 128).transpose(2, 0, 1)
    return {
        "wr%d" % L: np.ascontiguousarray(inp["router_w"][L]), "br%d" % L: np.ascontiguousarray(inp["router_b"][L][None, :]),
        "w1g%d" % L: np.ascontiguousarray(w1[:, :, 0::2]), "w1l%d" % L: np.ascontiguousarray(w1[:, :, 1::2]),
        "w2%d" % L: np.ascontiguousarray(inp["moe_w2"][L]),
        "b1%d" % L: np.ascontiguousarray(np.concatenate([b1g, b1l], axis=2)), "b2%d" % L: np.ascontiguousarray(inp["moe_b2"][L]),
    }


def host_inputs_B1(inp, gs):
    maps = []
    mh = moe_host(inp, 0)
    lnp = np.stack([inp["ln_g"][0, 0], inp["ln_b"][0, 0], inp["ln_g"][0, 1], inp["ln_b"][0, 1]], axis=-1)
    lnp = np.ascontiguousarray(lnp.reshape(8, 128, 4).transpose(1, 0, 2))
    adaw = np.ascontiguousarray(inp["ada_w"][0][:, 2048:6144])
    adab = np.ascontiguousarray(inp["ada_b"][0][2048:6144].reshape(32, 128).T)
    w_out = np.ascontiguousarray(inp["ssd_w_out"][0])
    for core in range(8):
        b, q = core // 4, core % 4
        Gb = np.concatenate([np.asarray(gs[b * 4 + g]) for g in range(4)], axis=1)
        rows = np.concatenate([np.arange(CTX + q * 2048, CTX + (q + 1) * 2048), np.arange(q * 64, (q + 1) * 64)])
        gT = np.ascontiguousarray(Gb[rows].T)
        xT = np.concatenate([inp["x"][b, q * 2048:(q + 1) * 2048], inp["ctx"][b, q * 64:(q + 1) * 64]], axis=0).T
        cT = np.stack([inp["c"][b].reshape(8, 128).T, inp["c_ctx"].reshape(8, 128).T], axis=-1)
        m = {"xT": np.ascontiguousarray(xT, dtype=np.float32), "gT": gT, "w_out": w_out, "adaw": adaw, "adab": adab,
             "cT": np.ascontiguousarray(cT, dtype=np.float32), "lnp": lnp}
        m.update(mh)
        maps.append(m)
    return maps


def run_B1(inp, gs, experts=tuple(range(32))):
    nc = build_B1(experts)
    maps = host_inputs_B1(inp, gs)
    res = run_bass_kernel_spmd(nc, maps, core_ids=list(range(8)))
    return [r["outT"] for r in res.results]


B2_NT = 2560
B2_NL = 2304
B2_KV_TILES = [(0, 512, 0), (512, 512, 0), (1024, 512, 0), (1536, 512, 0), (2048, 256, 0), (2304, 256, 1)]
B2_OWN_TILES = [(128 + i * 512, 512, 0) for i in range(4)]


def build_B2(experts=tuple(range(32)), dbg=False):
    nc = bass.Bass("TRN2", target_bir_lowering=False)
    dr = lambda name, shape, dt=F32: nc.dram_tensor(name, list(shape), dt, kind="ExternalInput").ap()
    NT = B2_NT
    xT = dr("xT", [D, NT])
    adaw = dr("adaw", [D, 6144])
    adab = dr("adab", [128, 48])
    cT = dr("cT", [128, 8, 2])
    lnp_d = dr("lnp", [128, 8, 4])
    wq_d = dr("wq", [D, 1024])
    wqs_d = dr("wqs", [D, 1024])
    bq_d = dr("bq", [128, 8, 2])
    wkd_d = dr("wkd", [D, 512])
    wkds_d = dr("wkds", [D, 512])
    bk_d = dr("bk", [128, 4, 2])
    wv_d = dr("wv", [D, 256])
    bv_d = dr("bv", [1, 256])
    cos_d = dr("cosT", [128, B2_NL])
    sin_d = dr("sinT", [128, B2_NL])
    sink_d = dr("sinks", [1, 16])
    mask_d = dr("masks", [3, 128, 384])
    wo_d = dr("wo", [D, D])
    bo_d = dr("bo", [128, 8])
    md = moe_dram(nc, "1", 32)
    md["gsc"] = nc.dram_tensor("gsc", [32, 2048], F32, kind="Internal").ap()
    outT = nc.dram_tensor("outT", [D, 2048], F32, kind="ExternalOutput").ap()
    dbg_out = {}
    with contextlib.ExitStack() as es:
        S = Sched(nc, es)
        ones, onesT, identf, identfT, epsc, epscT = common_consts(S)
        (idb, idbT) = mk(S, "idb", [128, 128], BF16)
        S.op(S.dve, [identfT], [idbT], lambda: nc.vector.tensor_copy(idb[:], identf[:]))
        P = [mkp(S, "P%d" % i) for i in range(8)]
        (X, _) = mk(S, "X", [128, 8, NT], F32)
        Xk = [T("Xk%d" % i) for i in range(len(B2_KV_TILES))]
        for ti, (t0, n, v) in enumerate(B2_KV_TILES):
            S.dma(S.sp, "ld", X[:, :, t0:t0 + n], xT[:, t0:t0 + n].rearrange("(c p) t -> p c t", p=128), [], [Xk[ti]])
        Xt = [T("Xo%d" % i) for i in range(4)]
        (lnp, lnpT) = mk(S, "lnp", [128, 8, 4], F32)
        S.dma(S.sp, "misc", lnp[:], lnp_d, [], [lnpT])
        (mod, modT) = mk(S, "mod", [128, 48, 2], F32)
        with contextlib.ExitStack() as es2:
            emit_ada(S, es2, adaw, adab, cT, 6144, mod, modT, P[7][0], P[7][1], "B2")
            drain(S, [modT])
        (msc, mscT) = mk(S, "msc1", [128, 8, 2], F32)
        S.op(S.dve, [modT], [mscT], lambda: nc.vector.tensor_scalar(out=msc[:], in0=mod[:, 8:16, :], scalar1=1.0, scalar2=None, op0=ALU.add))
        with contextlib.ExitStack() as esA:
            (K, KT) = mk(S, "K", [128, 4, NT], BF16, es=esA)
            (V, VT) = mk(S, "V", [128, NT // 128, 256], BF16, es=esA)
            (Q, QT) = mk(S, "Q", [128, 8, 2048], BF16, es=esA)
            with contextlib.ExitStack() as esQ:
                h_r = mk(S, "hTq", [128, 8, 512], BF16, n=2, es=esQ)
                cs_r = mk(S, "cosr", [128, 512], F32, n=2, es=esQ)
                sn_r = mk(S, "sinr", [128, 512], F32, n=2, es=esQ)
                t1_r = mk(S, "t1", [128, 512], F32, n=2, es=esQ)
                t2_r = mk(S, "t2", [128, 512], F32, n=2, es=esQ)
                (bq, bqT) = mk(S, "bq", [128, 8, 2], F32, es=esQ)
                (bk, bkT) = mk(S, "bk", [128, 4, 2], F32, es=esQ)
                (bvb, bvbT) = mk(S, "bvb", [128, 256], F32, es=esQ)
                S.dma(S.sp, "misc", bq[:], bq_d, [], [bqT])
                S.dma(S.sp, "misc", bk[:], bk_d, [], [bkT])
                S.dma(S.sp, "misc", bvb[:], bv_d.partition_broadcast(128), [], [bvbT])
                used = [bqT, bkT, bvbT]

                def modulate(t0, n, v, reads):
                    (hT, hTT) = h_r.next()

                    def modl():
                        li = None
                        for c in range(8):
                            li = nc.scalar.activation(out=hT[:, c, 0:n], in_=X[:, c, t0:t0 + n], func=AF.Identity, bias=mod[:, c, v:v + 1], scale=msc[:, c, v:v + 1])
                        return li
                    S.op(S.act, reads + [modT, mscT], [hTT], modl)
                    return hT, hTT

                def rope_evac(pa, pb_, bias_ap, bias_s_ap, cs, sn, csT, snT, out_ap, outT_, n):
                    (t1, t1T) = t1_r.next()
                    (t2, t2T) = t2_r.next()
                    S.op(S.dve, [pa[1], csT], [t1T], lambda: nc.vector.scalar_tensor_tensor(out=t1[:, 0:n], in0=pa[0][:, 0:n], scalar=bias_ap, in1=cs[:, 0:n], op0=ALU.add, op1=ALU.mult))
                    S.op(S.dve, [pb_[1], snT], [t2T], lambda: nc.vector.scalar_tensor_tensor(out=t2[:, 0:n], in0=pb_[0][:, 0:n], scalar=bias_s_ap, in1=sn[:, 0:n], op0=ALU.add, op1=ALU.mult))
                    S.op(S.pool, [t1T, t2T], [outT_], lambda: nc.gpsimd.tensor_tensor(out=out_ap, in0=t1[:, 0:n], in1=t2[:, 0:n], op=ALU.add))
                    used.extend([t1T, t2T])

                with contextlib.ExitStack() as esW:
                    (wkd, wkdT) = mk(S, "wkd", [128, 8, 512], BF16, es=esW)
                    (wkds, wkdsT) = mk(S, "wkds", [128, 8, 512], BF16, es=esW)
                    (wv, wvT) = mk(S, "wv", [128, 8, 256], BF16, es=esW)
                    S.dma(S.pool, "w", wkd[:], wkd_d.rearrange("(c p) n -> p c n", p=128), [], [wkdT])
                    S.dma(S.pool, "w", wkds[:], wkds_d.rearrange("(c p) n -> p c n", p=128), [], [wkdsT])
                    S.dma(S.pool, "w", wv[:], wv_d.rearrange("(c p) n -> p c n", p=128), [], [wvT])
                    pa_r = Ring([P[0], P[1]])
                    pb_r = Ring([P[2], P[3]])
                    pv_r = Ring([P[4], P[5]])
                    for ti, (t0, n, v) in enumerate(B2_KV_TILES):
                        hT, hTT = modulate(t0, n, v, [Xk[ti]])
                        if v == 0:
                            (cs, csT) = cs_r.next()
                            (sn, snT) = sn_r.next()
                            S.dma(S.sp, "tab", cs[:, 0:n], cos_d[:, t0:t0 + n], [], [csT])
                            S.dma(S.sp, "tab", sn[:, 0:n], sin_d[:, t0:t0 + n], [], [snT])
                            used.extend([csT, snT])
                        for g in range(4):
                            pa = pa_r.next()

                            def mk_(pa=pa, g=g, w=wkd):
                                li = None
                                for c in range(8):
                                    li = nc.tensor.matmul(pa[0][:, 0:n], w[:, c, g * 128:(g + 1) * 128], hT[:, c, 0:n], start=(c == 0), stop=(c == 7), skip_group_check=True)
                                return li
                            S.op(S.pe, [wkdT, hTT], [pa[1]], mk_)
                            if v == 0:
                                pb_ = pb_r.next()
                                S.op(S.pe, [wkdsT, hTT], [pb_[1]], lambda pb_=pb_, g=g: mk_(pb_, g, wkds))
                                rope_evac(pa, pb_, bk[:, g, 0:1], bk[:, g, 1:2], cs, sn, csT, snT, K[:, g, t0:t0 + n], KT, n)
                            else:
                                S.op(S.act, [pa[1], bkT], [KT], lambda pa=pa, g=g: nc.scalar.activation(out=K[:, g, t0:t0 + n], in_=pa[0][:, 0:n], func=AF.Identity,
                                                                                                        bias=bk[:, g, 0:1], scale=1.0))
                        for kb in range(n // 128):
                            pv = pv_r.next()

                            def mv(pv=pv, kb=kb):
                                li = None
                                for c in range(8):
                                    li = nc.tensor.matmul(pv[0][:, 0:256], hT[:, c, kb * 128:(kb + 1) * 128], wv[:, c, :], start=(c == 0), stop=(c == 7), skip_group_check=True)
                                return li
                            S.op(S.pe, [wvT, hTT], [pv[1]], mv)
                            S.op(S.dve, [pv[1], bvbT], [VT], lambda pv=pv, kb=kb: nc.vector.tensor_tensor(out=V[:, t0 // 128 + kb, :], in0=pv[0][:, 0:256], in1=bvb[:], op=ALU.add))
                    drain(S, [KT, VT, wkdT, wkdsT, wvT])
                for half in range(2):
                    with contextlib.ExitStack() as esW:
                        (wq, wqT) = mk(S, "wq%d" % half, [128, 8, 512], BF16, es=esW)
                        (wqs, wqsT) = mk(S, "wqs%d" % half, [128, 8, 512], BF16, es=esW)
                        hs = slice(half * 512, (half + 1) * 512)
                        S.dma(S.pool, "w", wq[:], wq_d[:, hs].rearrange("(c p) n -> p c n", p=128), [], [wqT])
                        S.dma(S.pool, "w", wqs[:], wqs_d[:, hs].rearrange("(c p) n -> p c n", p=128), [], [wqsT])
                        pa_r = Ring([P[0], P[1]])
                        pb_r = Ring([P[2], P[3]])
                        for i, (t0, n, v) in enumerate(B2_OWN_TILES):
                            hT, hTT = modulate(t0, n, v, Xk)
                            (cs, csT) = cs_r.next()
                            (sn, snT) = sn_r.next()
                            S.dma(S.sp, "tab", cs[:, 0:n], cos_d[:, t0:t0 + n], [], [csT])
                            S.dma(S.sp, "tab", sn[:, 0:n], sin_d[:, t0:t0 + n], [], [snT])
                            for cc in range(4):
                                c8 = half * 4 + cc
                                pa = pa_r.next()
                                pb_ = pb_r.next()

                                def mq(pp, w, cc=cc):
                                    li = None
                                    for c in range(8):
                                        li = nc.tensor.matmul(pp[0][:, 0:n], w[:, c, cc * 128:(cc + 1) * 128], hT[:, c, 0:n], start=(c == 0), stop=(c == 7), skip_group_check=True)
                                    return li
                                S.op(S.pe, [wqT, hTT], [pa[1]], lambda pa=pa: mq(pa, wq))
                                S.op(S.pe, [wqsT, hTT], [pb_[1]], lambda pb_=pb_: mq(pb_, wqs))
                                rope_evac(pa, pb_, bq[:, c8, 0:1], bq[:, c8, 1:2], cs, sn, csT, snT, Q[:, c8, i * 512:(i + 1) * 512], QT, n)
                        drain(S, [QT, wqT, wqsT])
                drain(S, [QT, KT, VT] + used + [x[1] for x in h_r.items])
            if dbg:
                S.dump("K", K[:], KT, BF16)
                S.dump("V", V[:], VT, BF16)
                S.dump("Q", Q[:], QT, BF16)
            with contextlib.ExitStack() as esT:
                (wo, woT) = mk(S, "wo", [128, 8, 1024], BF16, es=esT)
                S.dma(S.pool, "w", wo[:], wo_d.rearrange("(c p) n -> p c n", p=128), [], [woT])
                (msk, mskT) = mk(S, "msk", [128, 3, 384], F32, es=esT)
                S.dma(S.sp, "misc", msk[:], mask_d.rearrange("k p n -> p k n"), [], [mskT])
                (snk, snkT) = mk(S, "snk", [128, 16], F32, es=esT)
                S.dma(S.sp, "misc", snk[:], sink_d.partition_broadcast(128), [], [snkT])
                (bo, boT) = mk(S, "bo", [128, 8], F32, es=esT)
                S.dma(S.sp, "misc", bo[:], bo_d, [], [boT])
                S.op(S.dve, [boT, modT], [boT], lambda: nc.vector.tensor_tensor(out=bo[:], in0=bo[:], in1=mod[:, 16:24, 0], op=ALU.mult))
                sc_r = mk(S, "sc", [128, 640], F32, n=2, es=esT)
                p_r = mk(S, "pp", [128, 640], BF16, n=2, es=esT)
                pt_r = mk(S, "pt", [128, 640], BF16, n=2, es=esT)
                st_r = mk(S, "st", [128, 4], F32, n=3, es=esT)
                rd_r = mk(S, "rd", [128, 16], F32, n=2, es=esT)
                osb_r = mk(S, "osb", [128, 16, 64], BF16, n=2, es=esT)
                ot_r = mk(S, "ot", [128, 8, 512], BF16, n=2, es=esT)
                scA_r = Ring([P[0], P[1]])
                scB = P[2]
                pT = P[3]
                pO = [P[4], P[5]]
                pOT = P[6]
                pY = P[7]
                (ot, otT) = (None, None)
                for i in range(16):
                    kind = 0 if i == 0 else (2 if i == 15 else 1)
                    (rd, rdT) = rd_r.next()
                    (osb, osbT) = osb_r.next()
                    if i % 4 == 0:
                        (ot, otT) = ot_r.next()
                    for h in range(16):
                        g = h // 4
                        c8 = h // 2
                        pb0 = (h % 2) * 64
                        scA = scA_r.next()
                        Qh = Q[pb0:pb0 + 64, c8, i * 128:(i + 1) * 128]
                        S.op(S.pe, [QT, KT], [scA[1]], lambda: nc.tensor.matmul(scA[0][:, 0:384], Qh, K[pb0:pb0 + 64, g, i * 128:i * 128 + 384], start=True, stop=True, skip_group_check=True))
                        S.op(S.pe, [QT, KT], [scB[1]], lambda: nc.tensor.matmul(scB[0][:, 0:256], Qh, K[pb0:pb0 + 64, g, B2_NL:B2_NL + 256], start=True, stop=True, skip_group_check=True))
                        (sc, scT) = sc_r.next()
                        (st, stT) = st_r.next()
                        S.op(S.dve, [scA[1], mskT], [scT], lambda: nc.vector.scalar_tensor_tensor(out=sc[:, 0:384], in0=scA[0][:, 0:384], scalar=0.125, in1=msk[:, kind, :], op0=ALU.mult, op1=ALU.add))
                        S.op(S.act, [scB[1]], [scT], lambda: nc.scalar.activation(out=sc[:, 384:640], in_=scB[0][:, 0:256], func=AF.Copy, scale=0.125))
                        S.op(S.dve, [scT], [stT], lambda: nc.vector.reduce_max(out=st[:, 0:1], in_=sc[:], axis=AX.X))
                        S.op(S.dve, [stT, snkT], [stT], lambda: nc.vector.tensor_scalar(out=st[:, 1:2], in0=st[:, 0:1], scalar1=snk[:, h:h + 1], scalar2=-1.0, op0=ALU.max, op1=ALU.mult))
                        (pp, ppT) = p_r.next()
                        S.op(S.act, [scT, stT], [ppT, stT], lambda: nc.scalar.activation(out=pp[:], in_=sc[:], func=AF.Exp, bias=st[:, 1:2], scale=1.0, accum_out=st[:, 2:3]))
                        S.op(S.act, [snkT, stT], [stT], lambda: nc.scalar.activation(out=st[:, 3:4], in_=snk[:, h:h + 1], func=AF.Exp, bias=st[:, 1:2], scale=1.0))
                        S.op(S.dve, [stT], [stT], lambda: nc.vector.tensor_tensor(out=st[:, 2:3], in0=st[:, 2:3], in1=st[:, 3:4], op=ALU.add))
                        S.op(S.dve, [stT], [rdT], lambda: nc.vector.reciprocal(out=rd[:, h:h + 1], in_=st[:, 2:3]))
                        ptp = pT[0][:, 0:320].bitcast(BF16)

                        def trp():
                            li = None
                            for kb in range(5):
                                li = nc.tensor.transpose(ptp[:, kb * 128:(kb + 1) * 128], pp[:, kb * 128:(kb + 1) * 128], idb[:])
                            return li
                        S.op(S.pe, [ppT, idbT], [pT[1]], trp)
                        (pt, ptT) = pt_r.next()
                        S.op(S.act, [pT[1]], [ptT], lambda: nc.scalar.copy(out=pt[:], in_=ptp))
                        po = pO[h // 8]

                        def pv_():
                            li = None
                            for kb in range(5):
                                blk = (i + kb) if kb < 3 else (B2_NL // 128 + kb - 3)
                                li = nc.tensor.matmul(po[0][:, (h % 8) * 64:(h % 8 + 1) * 64], pt[:, kb * 128:(kb + 1) * 128], V[:, blk, g * 64:(g + 1) * 64],
                                                      start=(h % 8 == 0 and kb == 0), stop=(kb == 4), skip_group_check=True)
                            return li
                        S.op(S.pe, [ptT, VT], [po[1]], pv_)
                        if h % 8 == 7:
                            hb = (h // 8) * 8
                            S.op(S.dve, [po[1], rdT], [osbT], lambda: nc.vector.tensor_tensor(out=osb[:, hb:hb + 8, :], in0=po[0][:, :].rearrange("p (h e) -> p h e", e=64),
                                                                                              in1=bc(rd[:, hb:hb + 8].unsqueeze(2), [128, 8, 64]), op=ALU.mult))
                    otp = pOT[0][:, :].bitcast(BF16)
                    osf = osb[:].rearrange("p h e -> p (h e)")

                    def tro():
                        li = None
                        for k in range(8):
                            li = nc.tensor.transpose(otp[:, k * 128:(k + 1) * 128], osf[:, k * 128:(k + 1) * 128], idb[:])
                        return li
                    S.op(S.pe, [osbT, idbT], [pOT[1]], tro)
                    S.op(S.act, [pOT[1]], [otT], lambda: nc.scalar.copy(out=ot[:, :, (i % 4) * 128:(i % 4 + 1) * 128], in_=otp.rearrange("p (k q) -> p k q", q=128)))
                    if i % 4 == 3:
                        ti = i // 4
                        (t0, n, v) = B2_OWN_TILES[ti]
                        rds = [Xk[ti], Xk[ti + 1]]

                        def resid():
                            li = None
                            for m in range(8):
                                li = nc.scalar.activation(out=X[:, m, t0:t0 + n], in_=X[:, m, t0:t0 + n], func=AF.Identity, bias=bo[:, m:m + 1], scale=ALPHA)
                            return li
                        S.op(S.act, rds + [boT], [Xt[ti]], resid)
                        for m in range(8):
                            def mo(m=m):
                                li = None
                                for k in range(8):
                                    li = nc.tensor.matmul(pY[0][:, 0:n], wo[:, k, m * 128:(m + 1) * 128], ot[:, k, :], start=(k == 0), stop=(k == 7), skip_group_check=True)
                                return li
                            S.op(S.pe, [woT, otT], [pY[1]], mo)
                            S.op(S.dve, [pY[1], modT, Xt[ti]], [Xt[ti]], lambda m=m: nc.vector.scalar_tensor_tensor(out=X[:, m, t0:t0 + n], in0=pY[0][:, 0:n], scalar=mod[:, 16 + m, 0:1],
                                                                                                                   in1=X[:, m, t0:t0 + n], op0=ALU.mult, op1=ALU.add))
                allT = [x[1] for r in (sc_r, p_r, pt_r, st_r, rd_r, osb_r, ot_r) for x in r.items]
                drain(S, Xt + allT + [woT, mskT, snkT, boT, QT, KT, VT])
        tiles = B2_OWN_TILES
        if dbg:
            for ti, (t0, n, v) in enumerate(tiles):
                pass
        with contextlib.ExitStack() as es2:
            emit_ln(S, es2, X, Xt, tiles, lnp, lnpT, 0, 1, ones, onesT, epsc, epscT, P[2], P[3], "a")
            drain(S, Xt)
        with contextlib.ExitStack() as es2:
            used = emit_moe(S, es2, X, Xt, tiles, 2048, mod, modT, 24, 32, 40, P, md, experts, identf, identfT, "m1", hoff=128)
            drain(S, Xt + used)
        with contextlib.ExitStack() as es2:
            emit_ln(S, es2, X, Xt, tiles, lnp, lnpT, 2, 3, ones, onesT, epsc, epscT, P[2], P[3], "b")
            oT = T("out")
            for ti, (t0, n, v) in enumerate(tiles):
                S.dma(S.sp, "out", outT[:, t0 - 128:t0 - 128 + n].rearrange("(c p) t -> p c t", p=128), X[:, :, t0:t0 + n], [Xt[ti]], [oT])
            drain(S, Xt + [oT] + getattr(S, "dumps", []))
            for tok in S.out_toks:
                nc.sync.wait_ge(tok[0], tok[1])
    return nc


def rope_tables(q):
    i = np.arange(B2_NL)
    pos = q * 2048 - 128 + i
    pos = np.clip(pos, 0, SEQ - 1)
    row = (pos // 64).astype(np.float32)
    col = (pos % 64).astype(np.float32)
    nfreq = 16
    inv = np.power(np.float32(10000.0), -np.arange(nfreq, dtype=np.float32) / np.float32(nfreq)).astype(np.float32)
    ang = np.concatenate([row[:, None] * inv, col[:, None] * inv], axis=-1).astype(np.float32)
    cos = np.cos(ang).astype(np.float32)
    sin = np.sin(ang).astype(np.float32)
    p = np.arange(128)
    j = p % 64
    f = j % 32
    cosT = cos[:, f].T
    sgn = np.where(j < 32, -1.0, 1.0).astype(np.float32)
    sinT = sin[:, f].T * sgn[:, None]
    return np.ascontiguousarray(cosT, dtype=np.float32), np.ascontiguousarray(sinT, dtype=np.float32)


def host_inputs_B2(inp, x2_lat, x2_ctx):
    maps = []
    mh = moe_host(inp, 1)
    lnp = np.stack([inp["ln_g"][1, 0], inp["ln_b"][1, 0], inp["ln_g"][1, 1], inp["ln_b"][1, 1]], axis=-1)
    lnp = np.ascontiguousarray(lnp.reshape(8, 128, 4).transpose(1, 0, 2))
    adaw = np.ascontiguousarray(inp["ada_w"][1])
    adab = np.ascontiguousarray(inp["ada_b"][1].reshape(48, 128).T)
    wqkv = inp["attn_w_qkv"][0]
    bqkv = inp["attn_b_qkv"][0]
    swp = np.concatenate([np.arange(32, 64), np.arange(0, 32)])
    qcols = np.arange(1024)
    qs_cols = (qcols // 64) * 64 + swp[qcols % 64]
    wq = wqkv[:, :1024]
    wqs = wq[:, qs_cols]
    bqv = bqkv[:1024]
    bq = np.stack([bqv.reshape(8, 128).T, bqv[qs_cols].reshape(8, 128).T], axis=-1)
    kcols = np.concatenate([np.concatenate([np.arange(g * 64, (g + 1) * 64)] * 2) for g in range(4)])
    ks_cols = (kcols // 64) * 64 + swp[kcols % 64]
    wk = wqkv[:, 1024:1280]
    bkv = bqkv[1024:1280]
    wkd = wk[:, kcols]
    wkds = wk[:, ks_cols]
    bk = np.stack([bkv[kcols].reshape(4, 128).T, bkv[ks_cols].reshape(4, 128).T], axis=-1)
    wv = wqkv[:, 1280:1536]
    bv = bqkv[1280:1536][None, :]
    qi = np.arange(128)[:, None]
    kj = np.arange(384)[None, :]
    band = np.where(np.abs(qi + 128 - kj) <= 128, 0.0, NEG).astype(np.float32)
    for core in range(8):
        b, q = core // 4, core % 4
        xl = np.zeros((B2_NL, D), np.float32)
        lo = q * 2048 - 128
        hi = (q + 1) * 2048 + 128
        slo, shi = max(lo, 0), min(hi, SEQ)
        xl[slo - lo:shi - lo] = x2_lat[b, slo:shi]
        xT = np.concatenate([xl, x2_ctx[b]], axis=0).T
        cosT, sinT = rope_tables(q)
        m_first = band.copy()
        m_last = band.copy()
        if q == 0:
            m_first[:, 0:128] = NEG
        if q == 3:
            m_last[:, 256:384] = NEG
        masks = np.stack([m_first, band, m_last])
        cT = np.stack([inp["c"][b].reshape(8, 128).T, inp["c_ctx"].reshape(8, 128).T], axis=-1)
        m = {"xT": xT, "adaw": adaw, "adab": adab, "cT": cT, "lnp": lnp, "wq": wq, "wqs": wqs, "bq": bq, "wkd": wkd, "wkds": wkds, "bk": bk,
             "wv": wv, "bv": bv, "cosT": cosT, "sinT": sinT, "sinks": inp["attn_sinks"][0][None, :], "masks": masks,
             "wo": inp["attn_w_o"][0], "bo": inp["attn_b_o"][0].reshape(8, 128).T}
        m = {k: np.ascontiguousarray(v, dtype=np.float32) for k, v in m.items()}
        m.update(mh)
        maps.append(m)
    return maps


def run_B2(inp, x2_lat, x2_ctx, experts=tuple(range(32)), dbg=False):
    nc = build_B2(experts, dbg)
    maps = host_inputs_B2(inp, x2_lat, x2_ctx)
    res = run_bass_kernel_spmd(nc, maps, core_ids=list(range(8)))
    if dbg:
        return res.results
    return [r["outT"] for r in res.results]


def kernel(**inputs):
    inp = {k: np.asarray(v) for k, v in inputs.items()}
    gs = run_A(inp)
    o1 = run_B1(inp, gs)
    x2_lat = np.zeros((NB, SEQ, D), np.float32)
    x2_ctx = np.zeros((NB, CTX, D), np.float32)
    for core in range(8):
        b, q = core // 4, core % 4
        o = np.asarray(o1[core]).T
        x2_lat[b, q * 2048:(q + 1) * 2048] = o[:2048]
        x2_ctx[b, q * 64:(q + 1) * 64] = o[2048:]
    o2 = run_B2(inp, x2_lat, x2_ctx)
    out = np.zeros((NB, SEQ, D), np.float32)
    for core in range(8):
        b, q = core // 4, core % 4
        out[b, q * 2048:(q + 1) * 2048] = np.asarray(o2[core]).T
    return out
```
